# Optimizing a Trainium2 kernel written in Bass

```python
import jax, jax.numpy as jnp
from jax import lax
import numpy as np

D_MODEL = 1024
BATCH = 4
SEQ = 4096
DEPTH = 1

D_MIX = D_MODEL
POOL_WIDTH = D_MIX // 2
POOL_WINDOWS = (2, 4, 8, 16)
POOL_GROUPS = len(POOL_WINDOWS)
POOL_GROUP_DIM = POOL_WIDTH // POOL_GROUPS
GLA_WIDTH = D_MIX - POOL_WIDTH
GLA_HEADS = 4
GLA_DK_TOTAL = GLA_WIDTH // 2
GLA_DK = GLA_DK_TOTAL // GLA_HEADS
GLA_DV = GLA_WIDTH // GLA_HEADS
GLA_GATE_RANK = 16
GLA_GATE_NORMALIZER = 16.0
GLA_CHUNK = 16
IN_SIZES = (POOL_WIDTH, GLA_DK_TOTAL, GLA_DK_TOTAL, GLA_WIDTH, GLA_WIDTH, GLA_GATE_RANK)
D_IN = sum(IN_SIZES)
N_EXPERTS = 256
TOP_K = 8
N_GROUPS = 8
TOPK_GROUPS = 4
EXPERT_DIM = D_MODEL // 4
SHARED_DIM = EXPERT_DIM
ROUTED_SCALE = 2.5
MOE_BLOCK = 128
DEEPNORM_ALPHA = (2.0 * DEPTH) ** 0.25
DEEPNORM_BETA = (8.0 * DEPTH) ** -0.25
LN_EPS = 1e-5
RMS_EPS = 1e-5

kernel_name = 'hybrid_pool_gla_moe_deepnorm'

F32 = jnp.float32


def _layernorm(x, g, b):
    xf = x.astype(F32)
    mu = jnp.mean(xf, axis=-1, keepdims=True)
    var = jnp.mean(jnp.square(xf - mu), axis=-1, keepdims=True)
    return ((xf - mu) * lax.rsqrt(var + LN_EPS) * g.astype(F32) + b.astype(F32)).astype(x.dtype)


def _pool_mixer(p, pool_w_group, pool_scale):
    B, S, _ = p.shape
    pf = p.astype(F32).reshape(B, S, POOL_GROUPS, POOL_GROUP_DIM)
    c0 = jnp.concatenate([jnp.zeros((B, 1, POOL_GROUPS, POOL_GROUP_DIM), F32), jnp.cumsum(pf, axis=1)], axis=1)
    t = jnp.arange(S)
    means = []
    for g, w in enumerate(POOL_WINDOWS):
        cg = c0[:, :, g]
        lower = jnp.concatenate([jnp.zeros((B, w - 1, POOL_GROUP_DIM), F32), cg[:, :S + 1 - w]], axis=1)
        cnt = jnp.minimum(t + 1, w).astype(F32)
        means.append((cg[:, 1:] - lower) / cnt[None, :, None])
    mixed = jnp.stack(means, axis=2) - pf
    out = jnp.einsum('bsgc,gcd->bsgd', mixed, pool_w_group.astype(F32))
    out = out * pool_scale.astype(F32).reshape(POOL_GROUPS, POOL_GROUP_DIM)
    return out.reshape(B, S, POOL_WIDTH)


def _gla_mixer(q, k, v, r, g_low, gate_w2, gate_b, norm_w):
    B, S, _ = q.shape
    H, C = GLA_HEADS, GLA_CHUNK
    N = S // C
    q = q.astype(F32).reshape(B, N, C, H, GLA_DK) * (GLA_DK ** -0.5)
    k = k.astype(F32).reshape(B, N, C, H, GLA_DK)
    v = v.astype(F32).reshape(B, N, C, H, GLA_DV)
    gk = jax.nn.log_sigmoid(g_low.astype(F32) @ gate_w2.astype(F32) + gate_b.astype(F32)) / GLA_GATE_NORMALIZER
    b = jnp.cumsum(gk.reshape(B, N, C, H, GLA_DK), axis=2)
    causal = jnp.tril(jnp.ones((C, C), dtype=bool))
    diff = b[:, :, :, None] - b[:, :, None, :]
    decay = jnp.exp(jnp.where(causal[None, None, :, :, None, None], diff, -jnp.inf))
    scores = jnp.einsum('bnihd,bnjhd,bnijhd->bnhij', q, k, decay)
    o_intra = jnp.einsum('bnhij,bnjhe->bnihe', scores, v)
    b_last = b[:, :, -1]
    q_dec = q * jnp.exp(b)
    k_dec = k * jnp.exp(b_last[:, :, None] - b)
    kv = jnp.einsum('bnjhd,bnjhe->bnhde', k_dec, v)
    chunk_decay = jnp.exp(b_last)

    def step(state, inp):
        q_c, kv_c, dec_c = inp
        o = jnp.einsum('bihd,bhde->bihe', q_c, state)
        return state * dec_c[..., None] + kv_c, o

    state0 = jnp.zeros((B, H, GLA_DK, GLA_DV), F32)
    _, o_inter = lax.scan(step, state0, (jnp.moveaxis(q_dec, 1, 0), jnp.moveaxis(kv, 1, 0), jnp.moveaxis(chunk_decay, 1, 0)))
    o = (o_intra + jnp.moveaxis(o_inter, 0, 1)).reshape(B, S, H, GLA_DV)
    o = o * lax.rsqrt(jnp.mean(jnp.square(o), axis=-1, keepdims=True) + RMS_EPS) * norm_w.astype(F32)
    return o.reshape(B, S, GLA_WIDTH) * jax.nn.silu(r.astype(F32))


def _swiglu(x, wg, wu, wd):
    return (jax.nn.silu(x @ wg) * (x @ wu)) @ wd


def _route(h_flat, router_w, router_bias):
    T = h_flat.shape[0]
    scores = jax.nn.sigmoid(h_flat.astype(F32) @ router_w.astype(F32))
    biased = scores + router_bias.astype(F32)
    grp = biased.reshape(T, N_GROUPS, N_EXPERTS // N_GROUPS)
    grp_score = jnp.sum(lax.top_k(grp, 2)[0], axis=-1)
    _, top_grp = lax.top_k(grp_score, TOPK_GROUPS)
    grp_mask = jnp.sum(jax.nn.one_hot(top_grp, N_GROUPS, dtype=F32), axis=1) > 0
    expert_mask = jnp.repeat(grp_mask, N_EXPERTS // N_GROUPS, axis=1)
    _, idx = lax.top_k(jnp.where(expert_mask, biased, -jnp.inf), TOP_K)
    w = jnp.take_along_axis(scores, idx, axis=1)
    w = w / jnp.sum(w, axis=-1, keepdims=True) * ROUTED_SCALE
    return idx.astype(jnp.int32), w


def _routed_experts(h_flat, idx, w, wg, wu, wd):
    T, D = h_flat.shape
    A = T * TOP_K
    flat_e = idx.reshape(A)
    flat_tok = jnp.arange(A, dtype=jnp.int32) // TOP_K
    flat_w = w.reshape(A)
    order = jnp.argsort(flat_e)
    se, st, sw = flat_e[order], flat_tok[order], flat_w[order]
    counts = jnp.zeros((N_EXPERTS,), jnp.int32).at[flat_e].add(1)
    padded = (counts + MOE_BLOCK - 1) // MOE_BLOCK * MOE_BLOCK
    group_start = jnp.cumsum(counts) - counts
    padded_end = jnp.cumsum(padded)
    padded_start = padded_end - padded
    pos = padded_start[se] + jnp.arange(A, dtype=jnp.int32) - group_start[se]
    n_blocks = (A + N_EXPERTS * (MOE_BLOCK - 1)) // MOE_BLOCK
    P = n_blocks * MOE_BLOCK
    row_tok = jnp.full((P,), T, jnp.int32).at[pos].set(st)
    row_w = jnp.zeros((P,), F32).at[pos].set(sw)
    block_rows = jnp.arange(n_blocks, dtype=jnp.int32) * MOE_BLOCK
    block_e = jnp.minimum(jnp.searchsorted(padded_end, block_rows, side='right'), N_EXPERTS - 1).astype(jnp.int32)
    h_pad = jnp.concatenate([h_flat, jnp.zeros((1, D), h_flat.dtype)], axis=0)

    def block_fn(args):
        tok, e = args
        return _swiglu(h_pad[tok], wg[e], wu[e], wd[e])

    out = lax.map(block_fn, (row_tok.reshape(n_blocks, MOE_BLOCK), block_e))
    out = out.reshape(P, D).astype(F32) * row_w[:, None]
    return jax.ops.segment_sum(out, row_tok, num_segments=T + 1)[:T].astype(h_flat.dtype)


def setup_inputs(seed: int = 0) -> dict:
    key = jax.random.key(seed)
    ks = jax.random.split(key, 20)

    def nrm(k, shape, scale):
        return jax.random.normal(k, shape, F32) * scale

    beta = DEEPNORM_BETA
    col_scale = jnp.concatenate([
        jnp.full((POOL_WIDTH,), beta, F32), jnp.ones((2 * GLA_DK_TOTAL,), F32),
        jnp.full((GLA_WIDTH,), beta, F32), jnp.ones((GLA_WIDTH + GLA_GATE_RANK,), F32)])
    return {
        'x': nrm(ks[0], (BATCH, SEQ, D_MODEL), 1.0),
        'w_in': nrm(ks[1], (DEPTH, D_MODEL, D_IN), D_MODEL ** -0.5) * col_scale,
        'gla_gate_w2': nrm(ks[2], (DEPTH, GLA_GATE_RANK, GLA_DK_TOTAL), GLA_GATE_RANK ** -0.5),
        'gla_gate_b': nrm(ks[3], (DEPTH, GLA_DK_TOTAL), 0.1),
        'gla_norm_w': 1.0 + nrm(ks[4], (DEPTH, GLA_DV), 0.02),
        'pool_w_group': nrm(ks[5], (DEPTH, POOL_GROUPS, POOL_GROUP_DIM, POOL_GROUP_DIM), POOL_GROUP_DIM ** -0.5),
        'pool_scale': 1.0 + nrm(ks[6], (DEPTH, POOL_WIDTH), 0.1),
        'w_out': nrm(ks[7], (DEPTH, D_MIX, D_MODEL), D_MIX ** -0.5 * beta),
        'ln1_g': 1.0 + nrm(ks[8], (DEPTH, D_MODEL), 0.02),
        'ln1_b': nrm(ks[9], (DEPTH, D_MODEL), 0.02),
        'router_w': nrm(ks[10], (DEPTH, D_MODEL, N_EXPERTS), D_MODEL ** -0.5),
        'router_bias': nrm(ks[11], (DEPTH, N_EXPERTS), 0.01),
        'w_exp_gate': nrm(ks[12], (DEPTH, N_EXPERTS, D_MODEL, EXPERT_DIM), D_MODEL ** -0.5 * beta),
        'w_exp_up': nrm(ks[13], (DEPTH, N_EXPERTS, D_MODEL, EXPERT_DIM), D_MODEL ** -0.5 * beta),
        'w_exp_down': nrm(ks[14], (DEPTH, N_EXPERTS, EXPERT_DIM, D_MODEL), EXPERT_DIM ** -0.5 * beta),
        'w_sh_gate': nrm(ks[15], (DEPTH, D_MODEL, SHARED_DIM), D_MODEL ** -0.5 * beta),
        'w_sh_up': nrm(ks[16], (DEPTH, D_MODEL, SHARED_DIM), D_MODEL ** -0.5 * beta),
        'w_sh_down': nrm(ks[17], (DEPTH, SHARED_DIM, D_MODEL), SHARED_DIM ** -0.5 * beta),
        'ln2_g': 1.0 + nrm(ks[18], (DEPTH, D_MODEL), 0.02),
        'ln2_b': nrm(ks[19], (DEPTH, D_MODEL), 0.02),
    }


def reference(x, w_in, gla_gate_w2, gla_gate_b, gla_norm_w, pool_w_group, pool_scale, w_out, ln1_g, ln1_b,
              router_w, router_bias, w_exp_gate, w_exp_up, w_exp_down, w_sh_gate, w_sh_up, w_sh_down, ln2_g, ln2_b):
    B, S, D = x.shape
    offsets = np.cumsum(IN_SIZES)[:-1].tolist()
    h = x
    for l in range(DEPTH):
        proj = h @ w_in[l]
        p, q, k, v, r, g_low = jnp.split(proj, offsets, axis=-1)
        pool_out = _pool_mixer(p, pool_w_group[l], pool_scale[l])
        gla_out = _gla_mixer(q, k, v, r, g_low, gla_gate_w2[l], gla_gate_b[l], gla_norm_w[l])
        mixed = jnp.concatenate([pool_out, gla_out], axis=-1).astype(h.dtype) @ w_out[l]
        h = _layernorm(DEEPNORM_ALPHA * h + mixed, ln1_g[l], ln1_b[l])
        h_flat = h.reshape(B * S, D)
        idx, gate = _route(h_flat, router_w[l], router_bias[l])
        routed = _routed_experts(h_flat, idx, gate.astype(h.dtype), w_exp_gate[l], w_exp_up[l], w_exp_down[l])
        shared = _swiglu(h_flat, w_sh_gate[l], w_sh_up[l], w_sh_down[l])
        h = _layernorm(DEEPNORM_ALPHA * h + (routed + shared).reshape(B, S, D), ln2_g[l], ln2_b[l])
    return h
```

```python
import numpy as np
from contextlib import ExitStack
import concourse.bass as bass
import concourse.mybir as mybir
from concourse.bass_utils import run_bass_kernel_spmd

F32 = mybir.dt.float32
BF16 = mybir.dt.bfloat16
I32 = mybir.dt.int32
AF = mybir.ActivationFunctionType
ALU = mybir.AluOpType
AX = mybir.AxisListType

NCORES = 8
TL = 2048
NT = TL // 128
NE = 256
CAP = 192
ALPHA = 2.0 ** 0.25
LN_EPS = 1e-5
RMS_EPS = 1e-5
BIG = 1.0e9
PAD_IDX = 1 << 30


class Res:
    __slots__ = ("name", "last_write", "reads", "semgrp", "sem", "cnt", "excl")

    def __init__(self, name, semgrp=None, excl=False):
        self.name = name
        self.excl = excl
        self.last_write = None
        self.reads = []
        self.semgrp = semgrp if semgrp is not None else self
        self.sem = None
        self.cnt = 0


class Sched:
    ENGS = ("pe", "act", "dve", "pool", "sp")

    def __init__(self, nc, stack):
        self.nc = nc
        self.stack = stack
        self.q = {e: [] for e in self.ENGS}
        self.cnt = {e: 0 for e in self.ENGS}
        self.sem = {e: stack.enter_context(nc.semaphore("prog_" + e)) for e in self.ENGS}
        self.waited = {e: {} for e in self.ENGS}
        self.groups = []

    def _dma_grp(self, res):
        g = res.semgrp
        if g.sem is None:
            g.sem = self.stack.enter_context(self.nc.semaphore("dma_" + g.name))
            self.groups.append(g)
        return g

    def op(self, eng, fn, reads=(), writes=(), dma=False):
        xr = [r for r in reads if r.excl and r not in writes]
        if xr:
            reads = [r for r in reads if not r.excl]
            writes = list(writes) + xr
        deps = set()
        for r in reads:
            if r.last_write is not None:
                deps.add(r.last_write)
        for w in writes:
            if w.last_write is not None:
                deps.add(w.last_write)
            deps.update(w.reads)
        if dma:
            g = self._dma_grp(writes[0])
            g.cnt += 16
            token = (g.sem, g.cnt)
        else:
            self.cnt[eng] += 1
            token = (self.sem[eng], self.cnt[eng])
        if eng == "pe":
            deps = {d for d in deps if d[0] is not self.sem["pe"]}
        self.q[eng].append((fn, deps, token, dma))
        for r in reads:
            r.reads.append(token)
        for w in writes:
            w.last_write = token
            w.reads = []
        return token

    def barrier(self):
        toks = [(self.sem[e], self.cnt[e]) for e in self.ENGS if self.cnt[e] > 0]
        toks += [(g.sem, g.cnt) for g in self.groups]
        for e in self.ENGS:
            self.q[e].append((None, set(toks), None, False))

    def wait_final(self, eng, tokens):
        self.q[eng].append((None, set(tokens), None, False))

    def emit(self):
        nc = self.nc
        with nc.Block() as block:
            def run(engname, eng):
                waited = self.waited[engname]
                for (fn, deps, token, dma) in self.q[engname]:
                    best = {}
                    for (s, v) in deps:
                        if v > best.get(s.num, (None, 0))[1]:
                            best[s.num] = (s, v)
                    for k, (s, v) in best.items():
                        if waited.get(k, 0) >= v:
                            continue
                        eng.wait_ge(s, v)
                        waited[k] = v
                    if fn is None:
                        continue
                    ins = fn(eng)
                    ins.then_inc(token[0], 16 if dma else 1)
                self.q[engname] = []

            @block.tensor
            def _(e):
                run("pe", e)

            @block.scalar
            def _(e):
                run("act", e)

            @block.vector
            def _(e):
                run("dve", e)

            @block.gpsimd
            def _(e):
                run("pool", e)

            @block.sync
            def _(e):
                run("sp", e)


def build(stage="full", n_exp=NE):
    nc = bass.Bass("TRN2", target_bir_lowering=False)

    def din(name, shape, dt=F32):
        return nc.dram_tensor(name, list(shape), dt, kind="ExternalInput").ap()

    xT = din("xT", [1024, 2 * TL])
    xtok = din("xtok", [TL, 1024])
    w_in = din("w_in", [1024, 2064])
    wgl = din("wgl", [1024, 128])
    w2p = din("w2p", [128, 256])
    normw = din("normw", [128, 1])
    poolw = din("poolw", [4, 128, 128])
    pscale = din("pscale", [128, 4])
    w_out = din("w_out", [1024, 1024])
    ln1g = din("ln1g", [128, 1024])
    ln1b = din("ln1b", [128, 1024])
    ln2g = din("ln2g", [128, 1024])
    ln2b = din("ln2b", [128, 1024])
    router_w = din("router_w", [1024, 256])
    rbias = din("rbias", [128, 256])
    if stage == "full":
        weg = din("weg", [NE, 1024, 256])
        weu = din("weu", [NE, 1024, 256])
        wed = din("wed", [NE, 256, 1024])
    if stage == "route":
        dbg_slot = nc.dram_tensor("dbg_slot", [128, NT * 8], F32, kind="ExternalOutput").ap()
        dbg_w8 = nc.dram_tensor("dbg_w8", [128, NT * 8], F32, kind="ExternalOutput").ap()
        dbg_tbl = nc.dram_tensor("dbg_tbl", [128, 2 * NE], I32, kind="ExternalOutput").ap()
    wsg = din("wsg", [1024, 256])
    wsu = din("wsu", [1024, 256])
    wsd = din("wsd", [256, 1024])
    c_mcur = din("c_mcur", [128, 4, 128])
    c_mprev = din("c_mprev", [128, 4, 128])
    c_mfirst = din("c_mfirst", [128, 4, 128])
    c_uinc = din("c_uinc", [128, 128])
    c_rgt = din("c_rgt", [128, 128])
    c_mask4 = din("c_mask4", [128, 4, 128])
    c_ident = din("c_ident", [128, 128])
    c_ustrict = din("c_ustrict", [128, 128])
    c_ones = din("c_ones", [128, 128])
    c_iota = din("c_iota", [128, 256])
    c_tokid = din("c_tokid", [128, NT, 2], I32)
    c_tblinit = din("c_tblinit", [128, 512], I32)
    out = nc.dram_tensor("out", [TL, 1024], F32, kind="ExternalOutput").ap()

    hbf = nc.dram_tensor("hbf", [TL + 1, 1024], BF16).ap()
    zacc = nc.dram_tensor("zacc", [TL, 1024], F32).ap()
    slot_tok = nc.dram_tensor("slot_tok", [CAP * NE, 2], I32).ap()
    yslots = nc.dram_tensor("yslots", [CAP * NE, 1024], F32).ap()

    with ExitStack() as st0:
        S = Sched(nc, st0)

        def mk(st):
            def sb(name, shape, dt=F32):
                return st.enter_context(nc.sbuf_tensor(name, list(shape), dt))
            return sb

        sb0 = mk(st0)

        def MM(o, lhsT, rhs, start, stop, r, w):
            S.op("pe", lambda e: e.matmul(o, lhsT, rhs, start=start, stop=stop), r, w)

        def TR(o, in_, ident, r, w):
            S.op("pe", lambda e: e.transpose(o, in_, ident), r, w)

        def ACT(o, in_, func, r, w, **kw):
            return S.op("act", lambda e: e.activation(out=o, in_=in_, func=func, **kw), r, w)

        def TT(eng, o, a, b, op, r, w):
            return S.op(eng, lambda e: e.tensor_tensor(out=o, in0=a, in1=b, op=op), r, w)

        def TS(eng, o, a, s1, s2, op0, op1, r, w, **kw):
            if op1 is None:
                return S.op(eng, lambda e: e.tensor_scalar(out=o, in0=a, scalar1=s1, scalar2=None, op0=op0, **kw), r, w)
            return S.op(eng, lambda e: e.tensor_scalar(out=o, in0=a, scalar1=s1, scalar2=s2, op0=op0, op1=op1, **kw), r, w)

        def STT(o, a, s, b, op0, op1, r, w, **kw):
            return S.op("dve", lambda e: e.scalar_tensor_tensor(out=o, in0=a, scalar=s, in1=b, op0=op0, op1=op1, **kw), r, w)

        def CP(eng, o, a, r, w):
            return S.op(eng, lambda e: e.tensor_copy(out=o, in_=a), r, w)

        def DMA(q, o, in_, r, w, **kw):
            return S.op(q, lambda e: e.dma_start(out=o, in_=in_, **kw), r, w, dma=True)

        bc_reg = {}

        def BCG(e):
            if "g" not in bc_reg:
                bc_reg["g"] = e.alloc_register("bcg")
                e.reg_mov(bc_reg["g"], TL - 1)
            return bc_reg["g"]

        def BC(e):
            if "r" not in bc_reg:
                bc_reg["r"] = e.alloc_register("bc")
                e.reg_mov(bc_reg["r"], CAP * NE - 1)
            return bc_reg["r"]

        banks = []
        for i in range(8):
            t = st0.enter_context(nc.psum_tensor(f"bank{i}", [128, 512], F32))
            banks.append((t, Res(f"bank{i}", excl=True)))
        bk = [0]

        def PS():
            b = banks[bk[0] % 8]
            bk[0] += 1
            return b

        def const_load(name, src, shape, dt=F32, q="sp"):
            t = sb0(name, shape, dt)
            r = Res(name)
            DMA(q, t[:], src, [], [r])
            return t, r

        ident32, r_ident32 = const_load("ident32", c_ident, [128, 128])
        identb, r_identb = const_load("identb", c_ident, [128, 128], BF16, "pool")
        tokid, r_tokid = const_load("tokid", c_tokid, [128, NT, 2], I32)
        slot8f = sb0("slot8f", [128, NT * 8]); r_slot8f = Res("slot8f")
        slot8i = sb0("slot8i", [128, NT * 8], I32); r_slot8i = Res("slot8i")
        w8 = sb0("w8", [128, NT * 8]); r_w8 = Res("w8")
        g_out = Res("outgrp")
        g_out2 = [Res("outgrp0"), Res("outgrp1")]
        g_hbf = Res("hbf")
        g_zacc = Res("zacc")
        g_slot = Res("slot_tok")
        r_slotinit = Res("slot_init")

        ln_eng = ["dve"]

        def layer_norm(z, r_z, gam, r_gam, bet, r_bet, o, r_o, tmp, r_tmp, st6, r_st6, mv, r_mv, sm, r_sm):
            S.op("dve", lambda e: e.bn_stats(out=st6[:, 0, :], in_=z[:, 0:512]), [r_z], [r_st6])
            S.op("dve", lambda e: e.bn_stats(out=st6[:, 1, :], in_=z[:, 512:1024]), [r_z], [r_st6])
            S.op("dve", lambda e: e.bn_aggr(out=mv[:, :], in_=st6[:, :, :]), [r_st6], [r_mv])
            ACT(sm[:, 0:1], mv[:, 1:2], AF.Ln, [r_mv], [r_sm], bias=LN_EPS)
            ACT(sm[:, 1:2], sm[:, 0:1], AF.Exp, [r_sm], [r_sm], scale=-0.5)
            TS("dve", sm[:, 2:3], mv[:, 0:1], sm[:, 1:2], -1.0, ALU.mult, ALU.mult, [r_mv, r_sm], [r_sm])
            ACT(o[:, :], z[:, :], AF.Identity, [r_z, r_sm], [r_o], scale=sm[:, 1:2], bias=sm[:, 2:3])
            TT(ln_eng[0], o[:, :], o[:, :], gam[:, :], ALU.mult, [r_o, r_gam], [r_o])
            TT(ln_eng[0], o[:, :], o[:, :], bet[:, :], ALU.add, [r_o, r_bet], [r_o])

        with ExitStack() as st1:
            sb = mk(st1)
            w_in_bf = sb("w_in_bf", [128, 8, 2064], BF16)
            r_win = [Res("win_a"), Res("win_b")]
            w_in_v = w_in.rearrange("(c p) n -> p c n", p=128)
            DMA("pool", w_in_bf[:, :, 0:1024], w_in_v[:, :, 0:1024], [], [r_win[0]])
            DMA("pool", w_in_bf[:, :, 1024:2064], w_in_v[:, :, 1024:2064], [], [r_win[1]])
            wgl_bf = sb("wgl_bf", [128, 8, 128], BF16); r_wgl = Res("wgl")
            DMA("pool", wgl_bf[:], wgl.rearrange("(c p) n -> p c n", p=128), [], [r_wgl])
            mcur, r_mcur = sb("mcur", [128, 4, 128], BF16), Res("mcur")
            DMA("pool", mcur[:], c_mcur, [], [r_mcur])
            mprev, r_mprev = sb("mprev", [128, 4, 128], BF16), Res("mprev")
            DMA("pool", mprev[:], c_mprev, [], [r_mprev])
            mfirst, r_mfirst = sb("mfirst", [128, 4, 128], BF16), Res("mfirst")
            DMA("pool", mfirst[:], c_mfirst, [], [r_mfirst])
            poolw_bf, r_poolw = sb("poolw_bf", [128, 4, 128], BF16), Res("poolw")
            DMA("pool", poolw_bf[:], poolw.rearrange("g c d -> c g d"), [], [r_poolw])
            w_out_bf, r_wout = sb("w_out_bf", [128, 8, 1024], BF16), Res("wout")
            DMA("pool", w_out_bf[:], w_out.rearrange("(c p) n -> p c n", p=128), [], [r_wout])
            wsgu, r_wsgu = sb("wsgu", [128, 8, 512], BF16), [Res("wsg"), Res("wsu")]
            DMA("pool", wsgu[:, :, 0:256], wsg.rearrange("(c p) n -> p c n", p=128), [], [r_wsgu[0]])
            DMA("pool", wsgu[:, :, 256:512], wsu.rearrange("(c p) n -> p c n", p=128), [], [r_wsgu[1]])
            wsd_bf, r_wsd = sb("wsd_bf", [128, 2, 1024], BF16), Res("wsd")
            DMA("pool", wsd_bf[:], wsd.rearrange("(c p) n -> p c n", p=128), [], [r_wsd])
            rw_sb, r_rw = sb("rw_sb", [128, 8, 256]), Res("rw")
            DMA("sp", rw_sb[:], router_w.rearrange("(c p) n -> p c n", p=128), [], [r_rw])
            uinc, r_uinc = sb("uinc", [128, 128]), Res("uinc")
            DMA("sp", uinc[:], c_uinc, [], [r_uinc])
            rgt, r_rgt = sb("rgt", [128, 128]), Res("rgt")
            DMA("sp", rgt[:], c_rgt, [], [r_rgt])
            mask4, r_mask4 = sb("mask4", [128, 4, 128], BF16), Res("mask4")
            DMA("pool", mask4[:], c_mask4, [], [r_mask4])
            ustrict, r_ustrict = sb("ustrict", [128, 128]), Res("ustrict")
            DMA("sp", ustrict[:], c_ustrict, [], [r_ustrict])
            ones32, r_ones = sb("ones32", [128, 128]), Res("ones")
            DMA("sp", ones32[:], c_ones, [], [r_ones])
            iota_e, r_iota = sb("iota_e", [128, 256]), Res("iota")
            DMA("sp", iota_e[:], c_iota, [], [r_iota])
            w2p_sb, r_w2p = sb("w2p_sb", [128, 256]), Res("w2p")
            DMA("sp", w2p_sb[:], w2p, [], [r_w2p])
            normw_sb, r_normw = sb("normw_sb", [128, 1]), Res("normw")
            DMA("sp", normw_sb[:], normw, [], [r_normw])
            pscale_sb, r_pscale = sb("pscale_sb", [128, 4]), Res("pscale")
            DMA("sp", pscale_sb[:], pscale, [], [r_pscale])
            rbias_sb, r_rbias = sb("rbias_sb", [128, 256]), Res("rbias")
            DMA("sp", rbias_sb[:], rbias, [], [r_rbias])
            g1, r_g1 = sb("g1", [128, 1024]), Res("g1")
            DMA("sp", g1[:], ln1g, [], [r_g1])
            b1, r_b1 = sb("b1", [128, 1024]), Res("b1")
            DMA("sp", b1[:], ln1b, [], [r_b1])
            tblinit, r_tblinit = sb("tblinit", [128, 512], I32), Res("tblinit")
            DMA("sp", tblinit[:], c_tblinit, [], [r_tblinit])
            st_v = slot_tok.rearrange("(s e) o -> s (e o)", e=NE)
            DMA("sp", st_v[0:128, :], tblinit[:], [r_tblinit], [r_slotinit])
            r_slotinit_b = Res("slot_init_b")
            DMA("sp", st_v[128:CAP, :], tblinit[0:CAP - 128, :], [r_tblinit], [r_slotinit_b])

            xTb = [sb(f"xTb{i}", [128, 8, 512], BF16) for i in range(2)]
            r_xTb = [Res(f"xTb{i}") for i in range(2)]
            glT, r_glT = sb("glT", [128, 512]), Res("glT")
            S.op("dve", lambda e: e.memset(glT[:], 0.0), [], [r_glT])
            S.op("dve", lambda e: e.memset(glT[32:33, :], 1.0), [], [r_glT])
            DMA("sp", hbf[TL:TL + 1, :], glT[64:65, :].bitcast(BF16), [r_glT], [Res("hbf_zero", semgrp=g_hbf)])
            vtok = [sb(f"vtok{i}", [128, 512], BF16) for i in range(2)]
            r_vtok = [Res(f"vtok{i}") for i in range(2)]
            ptok = [sb(f"ptok{i}", [128, 512], BF16) for i in range(2)]
            r_ptok = [Res(f"ptok{i}") for i in range(2)]
            S.op("dve", lambda e: e.memset(ptok[0][:], 0.0), [], [r_ptok[0]])
            S.op("dve", lambda e: e.memset(ptok[1][:], 0.0), [], [r_ptok[1]])
            ez, r_ez = sb("ez", [128, 256]), Res("ez")
            sp_, r_sp = sb("sp_", [128, 256]), Res("sp_")
            eb, r_eb = sb("eb", [128, 256]), Res("eb")
            kdec, r_kdec = sb("kdec", [128, 256], BF16), Res("kdec")
            dec, r_dec = sb("dec", [128, 4]), Res("dec")
            S32, r_S32 = sb("S32", [128, 2, 256]), Res("S32")
            S.op("dve", lambda e: e.memset(S32[:], 0.0), [], [r_S32])
            Sbf, r_Sbf = sb("Sbf", [128, 2, 256], BF16), Res("Sbf")
            qT32, r_qT32 = sb("qT32", [128, 2, 512]), Res("qT32")
            kT32, r_kT32 = sb("kT32", [128, 2, 512]), Res("kT32")
            srT, r_srT = sb("srT", [128, 4, 512]), Res("srT")
            ebT, r_ebT = sb("ebT", [128, 256]), Res("ebT")
            enbT, r_enbT = sb("enbT", [128, 256]), Res("enbT")
            ktT, r_ktT = sb("ktT", [128, 2, 128], BF16), Res("ktT")
            qTz = [sb(f"qTz{i}", [128, 4, 128], BF16) for i in range(2)]
            r_qTz = [Res(f"qTz{i}") for i in range(2)]
            S.op("dve", lambda e: e.memset(qTz[0][:], 0.0), [], [r_qTz[0]])
            S.op("dve", lambda e: e.memset(qTz[1][:], 0.0), [], [r_qTz[1]])
            sTm, r_sTm = sb("sTm", [128, 4, 128], BF16), Res("sTm")
            osq, r_osq = sb("osq", [128, 512]), Res("osq")
            lnv, r_lnv = osq, r_osq
            rstd, r_rstd = osq, r_osq
            on_, r_on = sb("on_", [128, 512]), Res("on_")
            mixT, r_mixT = sb("mixT", [128, 4, 128], BF16), Res("mixT")
            catT, r_catT = sb("catT", [128, 8, 128], BF16), Res("catT")
            xt0 = sb("xt0", [128, 1024])
            xt = [xt0, xt0]
            r_xt0 = Res("xt0")
            r_xt = [r_xt0, r_xt0]
            z1, r_z1 = sb("z1", [128, 1024]), Res("z1")
            lnt, r_lnt = None, None
            st6, r_st6 = sb("st6", [128, 2, 6]), Res("st6")
            mv, r_mv = sb("mv", [128, 2]), Res("mv")
            sm, r_sm = sb("sm", [128, 4]), Res("sm")
            hring = [sb(f"h{i}", [128, 1024]) for i in range(4)]
            r_hring = [Res(f"h{i}") for i in range(4)]
            hb, r_hb = sb("hb", [128, 1024], BF16), Res("hb")
            hT32, r_hT32 = sb("hT32", [128, 8, 128]), Res("hT32")
            hTb, r_hTb = sb("hTb", [128, 8, 512], BF16), Res("hTb")
            scores2 = [sb(f"scores{i}", [128, 256]) for i in range(2)]
            r_scores2 = [Res(f"scores{i}") for i in range(2)]
            pending = [None]
            prev = [None, None, None]
            v8s, r_v8s = sb("v8s", [128, 8]), Res("v8s")
            biased, r_biased = sb("biased", [128, 256]), Res("biased")
            tmpA, r_tmpA = sb("tmpA", [128, 256]), Res("tmpA")
            tmpB, r_tmpB = sb("tmpB", [128, 256]), Res("tmpB")
            g8, r_g8 = sb("g8", [128, 32]), Res("g8")
            v8, r_v8 = sb("v8", [128, 8]), Res("v8")
            mbv, r_mbv = sb("mbv", [128, 256]), Res("mbv")
            sel, r_sel = sb("sel", [128, 256]), Res("sel")
            selcum, r_selcum = sb("selcum", [128, 256]), Res("selcum")
            S.op("dve", lambda e: e.memset(selcum[:], 0.0), [], [r_selcum])
            gs, r_gs = tmpB, r_tmpB
            rs, r_rs = sb("rs", [128, 2]), Res("rs")
            slotfull, r_slotfull = sb("slotfull", [128, 256]), Res("slotfull")
            junk, r_junk = tmpA, r_tmpA
            sg, r_sg = sb("sg", [128, 512]), Res("sg")
            actsh, r_actsh = sb("actsh", [128, 2, 512], BF16), Res("actsh")
            za0 = sb("za0", [128, 1024])
            za = [za0, za0]
            r_za0 = Res("za0")
            r_za = [r_za0, r_za0]

            xT_v = xT.rearrange("(c p) t -> p c t", p=128)

            def load_x_block(gblk):
                i = gblk % 2
                DMA("pool", xTb[i][:], xT_v[:, :, gblk * 512:(gblk + 1) * 512], [], [r_xTb[i]])
                return xTb[i], r_xTb[i]

            def proj_glow(xb, r_xb):
                t, r = PS()
                for kc in range(8):
                    MM(t[:, 0:512], wgl_bf[:, kc, :], xb[:, kc, :], kc == 0, kc == 7, [r_wgl, r_xb], [r])
                ACT(glT[0:32, :], t[0:32, 0:512], AF.Copy, [r], [r_glT])

            def tok_proj(xb, r_xb, cols, c0, c1, n):
                t, r = PS()
                for kc in range(8):
                    MM(t[:, 0:n], xb[:, kc, cols], w_in_bf[:, kc, c0:c1], kc == 0, kc == 7, [r_xb] + r_win, [r])
                return t, r

            def gate_and_kdec(cols, tk, r_tk):
                t, r = PS()
                MM(t[:, 0:256], glT[:, cols], w2p_sb[:, :], True, True, [r_glT, r_w2p], [r])
                ACT(ez[:, :], t[:, 0:256], AF.Exp, [r], [r_ez], scale=-1.0)
                ACT(sp_[:, :], ez[:, :], AF.Ln, [r_ez], [r_sp], bias=1.0)
                t2, r2 = PS()
                MM(t2[:, 0:256], rgt[:, :], sp_[:, :], True, True, [r_rgt, r_sp], [r2])
                ACT(eb[:, :], t2[:, 0:256], AF.Exp, [r2], [r_eb])
                TT("dve", kdec[:, :], tk[:, 0:256], eb[:, :], ALU.mult, [r_tk, r_eb], [r_kdec])

            def state_update(par, dec_ap_fn, r_decsrc):
                t, r = PS()
                for half in range(2):
                    MM(t[:, half * 256:(half + 1) * 256], kdec[:, half * 128:(half + 1) * 128],
                       vtok[par][:, half * 256:(half + 1) * 256], True, True, [r_kdec, r_vtok[par]], [r])
                for half in range(2):
                    STT(S32[:, half, :], S32[:, half, :], dec_ap_fn(half), t[:, half * 256:(half + 1) * 256],
                        ALU.mult, ALU.add, [r_S32, r_decsrc, r], [r_S32])

            gtile = 0
            for blk in range(4):
                xb, r_xb = load_x_block(blk)
                proj_glow(xb, r_xb)
                for t4 in range(4):
                    it = blk * 4 + t4
                    par = gtile % 2
                    gtile += 1
                    cols = slice(t4 * 128, (t4 + 1) * 128)
                    tv, r_tv = tok_proj(xb, r_xb, cols, 1024, 1536, 512)
                    tk, r_tk = tok_proj(xb, r_xb, cols, 768, 1024, 256)
                    ACT(vtok[par][:, :], tv[:, 0:512], AF.Copy, [r_tv], [r_vtok[par]])
                    gate_and_kdec(cols, tk, r_tk)
                    tb, r_tb = PS()
                    for half in range(2):
                        MM(tb[:, 2 * half:2 * half + 2], sp_[:, half * 128:(half + 1) * 128], uinc[:, 126:128],
                           True, True, [r_sp, r_uinc], [r_tb])
                    ACT(dec[:, :], tb[:, 0:4], AF.Exp, [r_tb], [r_dec])
                    state_update(par, lambda half: dec[:, 2 * half + 1:2 * half + 2], r_dec)
                    if it == 15:
                        tp, r_tp = tok_proj(xb, r_xb, cols, 0, 512, 512)
                        ACT(ptok[par][:, :], tp[:, 0:512], AF.Copy, [r_tp], [r_ptok[par]])
            ACT(Sbf[:, :, :], S32[:, :, :], AF.Copy, [r_S32], [r_Sbf])

            nxt = load_x_block(4)
            for blk in range(4):
                xb, r_xb = nxt
                if blk < 3:
                    nxt = load_x_block(4 + blk + 1)
                proj_glow(xb, r_xb)
                for m in range(2):
                    t, r = PS()
                    for kc in range(8):
                        MM(t[:, 0:512], w_in_bf[:, kc, 512 + m * 128:512 + (m + 1) * 128], xb[:, kc, :], kc == 0, kc == 7,
                           r_win + [r_xb], [r])
                    ACT(qT32[:, m, :], t[:, 0:512], AF.Copy, [r], [r_qT32])
                for m in range(2):
                    t, r = PS()
                    for kc in range(8):
                        MM(t[:, 0:512], w_in_bf[:, kc, 768 + m * 128:768 + (m + 1) * 128], xb[:, kc, :], kc == 0, kc == 7,
                           r_win + [r_xb], [r])
                    CP("dve", kT32[:, m, :], t[:, 0:512], [r], [r_kT32])
                for m in range(4):
                    t, r = PS()
                    for kc in range(8):
                        MM(t[:, 0:512], w_in_bf[:, kc, 1536 + m * 128:1536 + (m + 1) * 128], xb[:, kc, :], kc == 0, kc == 7,
                           r_win + [r_xb], [r])
                    ACT(srT[:, m, :], t[:, 0:512], AF.Silu, [r], [r_srT])

                for t4 in range(4):
                    it = blk * 4 + t4
                    par = gtile % 2
                    gtile += 1
                    cols = slice(t4 * 128, (t4 + 1) * 128)
                    hcur, r_hcur = hring[it % 4], r_hring[it % 4]
                    DMA("sp", xt[par][:, :], xtok[it * 128:(it + 1) * 128, :], [], [r_xt[par]])
                    tp, r_tp = tok_proj(xb, r_xb, cols, 0, 512, 512)
                    tv, r_tv = tok_proj(xb, r_xb, cols, 1024, 1536, 512)
                    tk, r_tk = tok_proj(xb, r_xb, cols, 768, 1024, 256)
                    ACT(ptok[par][:, :], tp[:, 0:512], AF.Copy, [r_tp], [r_ptok[par]])
                    ACT(vtok[par][:, :], tv[:, 0:512], AF.Copy, [r_tv], [r_vtok[par]])
                    gate_and_kdec(cols, tk, r_tk)
                    tb, r_tb = PS()
                    for half in range(2):
                        MM(tb[:, half * 128:(half + 1) * 128], sp_[:, half * 128:(half + 1) * 128], uinc[:, :],
                           True, True, [r_sp, r_uinc], [r_tb])
                    ACT(ebT[:, :], tb[:, 0:256], AF.Exp, [r_tb], [r_ebT])
                    ACT(enbT[:, :], tb[:, 0:256], AF.Exp, [r_tb], [r_enbT], scale=-1.0)
                    TT("dve", ktT[:, :, :], kT32[:, :, cols], enbT[:, :].rearrange("p (a t) -> p a t", a=2), ALU.mult,
                       [r_kT32, r_enbT], [r_ktT])
                    for h in range(4):
                        r0 = (h % 2) * 64
                        half = h // 2
                        STT(qTz[par][r0:r0 + 64, h, :], qT32[r0:r0 + 64, half, cols], 0.125,
                            ebT[r0:r0 + 64, half * 128:(half + 1) * 128], ALU.mult, ALU.mult,
                            [r_qT32, r_ebT], [r_qTz[par]])
                    ts_, r_ts = PS()
                    for h in range(4):
                        MM(ts_[:, h * 128:(h + 1) * 128], ktT[:, h // 2, :], qTz[par][:, h, :], True, True,
                           [r_ktT, r_qTz[par]], [r_ts])
                    TT("dve", sTm[:, :, :], ts_[:, 0:512].rearrange("p (a t) -> p a t", a=4), mask4[:, :, :], ALU.mult,
                       [r_ts, r_mask4], [r_sTm])
                    to, r_to = PS()
                    for h in range(4):
                        MM(to[:, h * 128:(h + 1) * 128], vtok[par][:, h * 128:(h + 1) * 128], sTm[:, h, :], True, False,
                           [r_vtok[par], r_sTm], [r_to])
                        MM(to[:, h * 128:(h + 1) * 128], Sbf[:, h // 2, (h % 2) * 128:(h % 2 + 1) * 128], qTz[par][:, h, :],
                           False, True, [r_Sbf, r_qTz[par]], [r_to])
                    ACT(osq[:, :], to[:, 0:512], AF.Square, [r_to], [r_osq])
                    tss, r_tss = PS()
                    MM(tss[:, 0:512], ones32[:, :], osq[:, :], True, True, [r_ones, r_osq], [r_tss])
                    ACT(lnv[:, :], tss[:, 0:512], AF.Ln, [r_tss], [r_lnv], scale=1.0 / 128.0, bias=RMS_EPS)
                    ACT(rstd[:, :], lnv[:, :], AF.Exp, [r_lnv], [r_rstd], scale=-0.5)
                    TT("dve", on_[:, :], to[:, 0:512], rstd[:, :], ALU.mult, [r_to, r_rstd], [r_on])
                    STT(catT[:, 4:8, :], on_[:, :].rearrange("p (a t) -> p a t", a=4), normw_sb[:, 0:1], srT[:, :, cols],
                        ALU.mult, ALU.mult, [r_on, r_normw, r_srT], [r_catT])
                    tpm, r_tpm = PS()
                    mc, r_mc = (mfirst, r_mfirst) if it == 0 else (mcur, r_mcur)
                    for g in range(4):
                        MM(tpm[:, g * 128:(g + 1) * 128], ptok[par][:, g * 128:(g + 1) * 128], mc[:, g, :], True, False,
                           [r_ptok[par], r_mc], [r_tpm])
                        MM(tpm[:, g * 128:(g + 1) * 128], ptok[1 - par][:, g * 128:(g + 1) * 128], mprev[:, g, :], False, True,
                           [r_ptok[1 - par], r_mprev], [r_tpm])
                    ACT(mixT[:, :, :], tpm[:, 0:512].rearrange("p (a t) -> p a t", a=4), AF.Copy, [r_tpm], [r_mixT])
                    tpo, r_tpo = PS()
                    for g in range(4):
                        MM(tpo[:, g * 128:(g + 1) * 128], poolw_bf[:, g, :], mixT[:, g, :], True, True, [r_poolw, r_mixT], [r_tpo])
                    for g in range(4):
                        TS("dve", catT[:, g, :], tpo[:, g * 128:(g + 1) * 128], pscale_sb[:, g:g + 1], None, ALU.mult, None,
                           [r_tpo, r_pscale], [r_catT])
                    state_update(par, lambda half: ebT[:, half * 128 + 127:half * 128 + 128], r_ebT)
                    ACT(Sbf[:, :, :], S32[:, :, :], AF.Copy, [r_S32], [r_Sbf])
                    def part2(it=it, par=par, hcur=hcur, r_hcur=r_hcur):
                        tm0, r_tm0 = PS()
                        tm1, r_tm1 = PS()
                        for kc in range(8):
                            MM(tm0[:, 0:512], catT[:, kc, :], w_out_bf[:, kc, 0:512], kc == 0, kc == 7, [r_catT, r_wout], [r_tm0])
                            MM(tm1[:, 0:512], catT[:, kc, :], w_out_bf[:, kc, 512:1024], kc == 0, kc == 7, [r_catT, r_wout], [r_tm1])
                        STT(z1[:, 0:512], xt[par][:, 0:512], ALPHA, tm0[:, 0:512], ALU.mult, ALU.add, [r_xt[par], r_tm0], [r_z1])
                        STT(z1[:, 512:1024], xt[par][:, 512:1024], ALPHA, tm1[:, 0:512], ALU.mult, ALU.add, [r_xt[par], r_tm1], [r_z1])
                        layer_norm(z1, r_z1, g1, r_g1, b1, r_b1, hcur, r_hcur, lnt, r_lnt, st6, r_st6, mv, r_mv, sm, r_sm)
                    def route_a(it=it, cols=cols, hcur=hcur, r_hcur=r_hcur):
                        ACT(hb[:, :], hcur[:, :], AF.Copy, [r_hcur], [r_hb])
                        DMA("sp", hbf[it * 128:(it + 1) * 128, :], hb[:, :], [r_hb], [Res(f"hbf{it}", semgrp=g_hbf)])
                        tt0, r_tt0 = PS()
                        tt1, r_tt1 = PS()
                        for c in range(8):
                            tt, r_tt = (tt0, r_tt0) if c < 4 else (tt1, r_tt1)
                            TR(tt[:, (c % 4) * 128:(c % 4 + 1) * 128], hcur[:, c * 128:(c + 1) * 128], ident32[:, :],
                               [r_hcur, r_ident32], [r_tt])
                        CP("dve", hT32[:, 0:4, :], tt0[:, 0:512].rearrange("p (a t) -> p a t", a=4), [r_tt0], [r_hT32])
                        CP("dve", hT32[:, 4:8, :], tt1[:, 0:512].rearrange("p (a t) -> p a t", a=4), [r_tt1], [r_hT32])
                        ACT(hTb[:, 0:4, cols], tt0[:, 0:512].rearrange("p (a t) -> p a t", a=4), AF.Copy, [r_tt0], [r_hTb])
                        ACT(hTb[:, 4:8, cols], tt1[:, 0:512].rearrange("p (a t) -> p a t", a=4), AF.Copy, [r_tt1], [r_hTb])
                        trt, r_trt = PS()
                        for kc in range(8):
                            MM(trt[:, 0:256], hT32[:, kc, :], rw_sb[:, kc, :], kc == 0, kc == 7, [r_hT32, r_rw], [r_trt])
                        scores, r_scores = scores2[it % 2], r_scores2[it % 2]
                        ACT(scores[:, :], trt[:, 0:256], AF.Sigmoid, [r_trt], [r_scores])
                    def route_b(it=it):
                        scores, r_scores = scores2[it % 2], r_scores2[it % 2]
                        TT("dve", biased[:, :], scores[:, :], rbias_sb[:, :], ALU.add, [r_scores, r_rbias], [r_biased])
                        b3 = biased[:, :].rearrange("p (g i) -> p g i", g=8)
                        S.op("dve", lambda e, b3=b3: e.tensor_reduce(out=g8[:, 0:8], in_=b3, axis=AX.X, op=ALU.max), [r_biased], [r_g8])
                        TT("dve", tmpA[:, :].rearrange("p (g i) -> p g i", g=8), b3, g8[:, 0:8].unsqueeze(2).to_broadcast([128, 8, 32]),
                           ALU.is_equal, [r_biased, r_g8], [r_tmpA])
                        STT(tmpB[:, :], tmpA[:, :], -BIG, biased[:, :], ALU.mult, ALU.add, [r_tmpA, r_biased], [r_tmpB])
                        tb3 = tmpB[:, :].rearrange("p (g i) -> p g i", g=8)
                        S.op("dve", lambda e, tb3=tb3: e.tensor_reduce(out=g8[:, 8:16], in_=tb3, axis=AX.X, op=ALU.max), [r_tmpB], [r_g8])
                        TT("dve", g8[:, 8:16], g8[:, 8:16], g8[:, 0:8], ALU.add, [r_g8], [r_g8])
                        S.op("dve", lambda e: e.max(out=g8[:, 16:24], in_=g8[:, 8:16]), [r_g8], [r_g8])
                        TS("dve", g8[:, 24:32], g8[:, 8:16], g8[:, 19:20], None, ALU.is_ge, None, [r_g8], [r_g8])
                        TS("dve", g8[:, 24:32], g8[:, 24:32], -1.0, BIG, ALU.add, ALU.mult, [r_g8], [r_g8])
                        TT("dve", mbv[:, :].rearrange("p (g i) -> p g i", g=8), b3, g8[:, 24:32].unsqueeze(2).to_broadcast([128, 8, 32]),
                           ALU.add, [r_biased, r_g8], [r_mbv])
                        S.op("dve", lambda e: e.max(out=v8[:, :], in_=mbv[:, :]), [r_mbv], [r_v8])
                        TS("dve", sel[:, :], mbv[:, :], v8[:, 7:8], None, ALU.is_ge, None, [r_mbv, r_v8], [r_sel])
                        TT("dve", gs[:, :], sel[:, :], scores[:, :], ALU.mult, [r_sel, r_scores], [r_gs])
                        S.op("dve", lambda e: e.max(out=v8s[:, :], in_=gs[:, :]), [r_gs], [r_v8s])
                        S.op("dve", lambda e: e.tensor_reduce(out=rs[:, 0:1], in_=v8s[:, :], axis=AX.X, op=ALU.add), [r_v8s], [r_rs])
                        S.op("dve", lambda e: e.reciprocal(out=rs[:, 1:2], in_=rs[:, 0:1]), [r_rs], [r_rs])
                        TS("dve", w8[:, it * 8:(it + 1) * 8], v8s[:, :], rs[:, 1:2], 2.5, ALU.mult, ALU.mult, [r_v8s, r_rs], [r_w8])
                        tps, r_tps = PS()
                        MM(tps[:, 0:256], ones32[:, :], selcum[:, :], True, False, [r_ones, r_selcum], [r_tps])
                        MM(tps[:, 0:256], ustrict[:, :], sel[:, :], False, True, [r_ustrict, r_sel], [r_tps])
                        STT(slotfull[:, :], tps[:, 0:256], float(NE), iota_e[:, :], ALU.mult, ALU.add, [r_tps, r_iota], [r_slotfull])
                        TT("dve", selcum[:, :], selcum[:, :], sel[:, :], ALU.add, [r_selcum, r_sel], [r_selcum])
                        for k in range(8):
                            col = it * 8 + k
                            STT(junk[:, :], gs[:, :], v8s[:, k:k + 1], slotfull[:, :], ALU.is_equal, ALU.mult,
                                [r_gs, r_v8s, r_slotfull], [r_junk, r_slot8f], accum_out=slot8f[:, col:col + 1])
                        CP("dve", slot8i[:, it * 8:(it + 1) * 8], slot8f[:, it * 8:(it + 1) * 8], [r_slot8f], [r_slot8i])
                        for k in range(8):
                            col = it * 8 + k
                            S.op("pool", lambda e, col=col, it=it: e.indirect_dma_start(
                                out=slot_tok, out_offset=bass.IndirectOffsetOnAxis(ap=slot8i[:, col:col + 1], axis=0),
                                in_=tokid[:, it, :], in_offset=None, bounds_check=BC(e), oob_is_err=False),
                                [r_slot8i, r_tokid, r_slotinit, r_slotinit_b], [Res(f"slot{col}", semgrp=g_slot)], dma=True)

                    if stage != "h" and prev[0] is not None:
                        prev[0]()
                        if prev[2] is not None:
                            prev[2]()
                    part2()
                    if stage == "h":
                        DMA("sp", out[it * 128:(it + 1) * 128, :], hcur[:, :], [r_hcur], [Res(f"out{it}", semgrp=g_out)])
                        continue
                    if prev[1] is not None:
                        prev[1]()
                    prev[0], prev[1], prev[2] = route_a, route_b, None
                def shared_blk(blk=blk):
                    for j in range(2):
                        tg, r_tg = PS()
                        tu, r_tu = PS()
                        for kc in range(8):
                            MM(tg[:, 0:512], wsgu[:, kc, j * 128:(j + 1) * 128], hTb[:, kc, :], kc == 0, kc == 7, r_wsgu + [r_hTb], [r_tg])
                        for kc in range(8):
                            MM(tu[:, 0:512], wsgu[:, kc, 256 + j * 128:256 + (j + 1) * 128], hTb[:, kc, :], kc == 0, kc == 7,
                               r_wsgu + [r_hTb], [r_tu])
                        ACT(sg[:, :], tg[:, 0:512], AF.Silu, [r_tg], [r_sg])
                        TT("dve", actsh[:, j, :], sg[:, :], tu[:, 0:512], ALU.mult, [r_sg, r_tu], [r_actsh])
                    for t4 in range(4):
                        it = blk * 4 + t4
                        cols = slice(t4 * 128, (t4 + 1) * 128)
                        hcur, r_hcur = hring[it % 4], r_hring[it % 4]
                        ty0, r_ty0 = PS()
                        ty1, r_ty1 = PS()
                        for kc in range(2):
                            MM(ty0[:, 0:512], actsh[:, kc, cols], wsd_bf[:, kc, 0:512], kc == 0, kc == 1, [r_actsh, r_wsd], [r_ty0])
                            MM(ty1[:, 0:512], actsh[:, kc, cols], wsd_bf[:, kc, 512:1024], kc == 0, kc == 1, [r_actsh, r_wsd], [r_ty1])
                        zp = it % 2
                        STT(za[zp][:, 0:512], hcur[:, 0:512], ALPHA, ty0[:, 0:512], ALU.mult, ALU.add, [r_hcur, r_ty0], [r_za[zp]])
                        STT(za[zp][:, 512:1024], hcur[:, 512:1024], ALPHA, ty1[:, 0:512], ALU.mult, ALU.add, [r_hcur, r_ty1], [r_za[zp]])
                        DMA("sp", zacc[it * 128:(it + 1) * 128, :], za[zp][:, :], [r_za[zp]], [Res(f"zacc{it}", semgrp=g_zacc)])

                if stage != "h":
                    prev[2] = shared_blk
            if stage != "h":
                prev[0]()
                prev[2]()
                prev[1]()
            S.barrier()
            S.emit()

        if stage == "h":
            return nc

        with ExitStack() as st2:
            sb = mk(st2)
            st_v = slot_tok.rearrange("(s e) o -> s (e o)", e=NE)
            tbl, r_tbl = sb("tbl", [128, 2 * NE], I32), Res("tbl")
            tblB, r_tblB = sb("tblB", [128, 2 * NE], I32), Res("tblB")
            DMA("sp", tblB[:], c_tblinit, [], [r_tblB])
            DMA("sp", tblB[0:CAP - 128, :], st_v[128:CAP, :], [], [r_tblB])
            g2, r_g2 = sb("g2", [128, 1024]), Res("g2")
            DMA("sp", g2[:], ln2g, [], [r_g2])
            b2, r_b2 = sb("b2", [128, 1024]), Res("b2")
            DMA("sp", b2[:], ln2b, [], [r_b2])
            DMA("sp", tbl[:], st_v[0:128, :], [], [r_tbl])
            if stage == "route":
                f1 = DMA("sp", dbg_tbl, tbl[:], [r_tbl, r_tblB], [Res("dbg_tbl")])
                f2 = DMA("sp", dbg_slot, slot8f[:], [r_slot8f], [Res("dbg_slot")])
                f3 = DMA("sp", dbg_w8, w8[:], [r_w8], [Res("dbg_w8")])
                zt_, r_zt = sb("zt_", [128, 1024]), Res("zt_")
                fl = [f1, f2, f3]
                for it in range(NT):
                    DMA("sp", zt_[:, :], zacc[it * 128:(it + 1) * 128, :], [], [r_zt])
                    fl.append(DMA("sp", out[it * 128:(it + 1) * 128, :], zt_[:, :], [r_zt], [Res(f"out{it}", semgrp=g_out)]))
                S.wait_final("sp", fl)
                S.emit()
                return nc
            NB = 3
            wgu = [sb(f"wgu{i}", [128, 8, 512], BF16) for i in range(NB)]
            r_wg = [Res(f"wg{i}") for i in range(NB)]
            r_wu = [Res(f"wu{i}") for i in range(NB)]
            wd = [sb(f"wd{i}", [128, 2, 1024], BF16) for i in range(NB)]
            r_wd = [Res(f"wd{i}") for i in range(NB)]
            xg = [sb(f"xg{i}", [128, 1024], BF16) for i in range(2)]
            r_xg = [Res(f"xg{i}") for i in range(2)]
            for i in range(2):
                S.op("dve", lambda e, i=i: e.memset(xg[i][:], 0.0), [], [r_xg[i]])
            xgT = [sb(f"xgT{i}", [128, 8, 128], BF16) for i in range(2)]
            r_xgT = [Res(f"xgT{i}") for i in range(2)]
            sge = [sb(f"sge{i}", [128, 256]) for i in range(2)]
            r_sge = [Res(f"sge{i}") for i in range(2)]
            ae = [sb(f"ae{i}", [128, 256], BF16) for i in range(2)]
            r_ae = [Res(f"ae{i}") for i in range(2)]
            aT = [sb(f"aT{i}", [128, 2, 128], BF16) for i in range(2)]
            r_aT = [Res(f"aT{i}") for i in range(2)]
            NY = 4
            ysb = [sb(f"ysb{i}", [128, 1024]) for i in range(NY)]
            r_ysb = [Res(f"ysb{i}") for i in range(NY)]
            g_ys = [Res(f"yslots{i}") for i in range(NY)]
            r_ys_all = []
            ys_v = yslots.rearrange("(s e) d -> s e d", e=NE)

            def load_w(e_):
                wb = e_ % NB
                DMA("pool", wgu[wb][:, :, 0:256], weg[e_].rearrange("(c p) n -> p c n", p=128), [], [r_wg[wb]])
                DMA("pool", wgu[wb][:, :, 256:512], weu[e_].rearrange("(c p) n -> p c n", p=128), [], [r_wu[wb]])
                DMA("pool", wd[wb][:], wed[e_].rearrange("(c p) n -> p c n", p=128), [], [r_wd[wb]])

            def stage_a(e_, blk, j):
                wb = e_ % NB
                p2 = j % 2
                tb_src, r_tb_src = (tbl, r_tbl) if blk == 0 else (tblB, r_tblB)
                S.op("pool", lambda e: e.indirect_dma_start(
                    out=xg[p2][:, :], out_offset=None, in_=hbf,
                    in_offset=bass.IndirectOffsetOnAxis(ap=tb_src[:, 2 * e_:2 * e_ + 1], axis=0),
                    bounds_check=BCG(e), oob_is_err=False),
                    [r_tb_src], [r_xg[p2]], dma=True)
                t, r = PS()
                tb_ = t[:, :].bitcast(BF16)
                for c in range(8):
                    TR(tb_[:, c * 128:(c + 1) * 128], xg[p2][:, c * 128:(c + 1) * 128], identb[:, :], [r_xg[p2], r_identb], [r])
                CP("dve", xgT[p2][:, :, :], tb_[:, 0:1024].rearrange("p (a t) -> p a t", a=8), [r], [r_xgT[p2]])
                t2, r2 = PS()
                for kc in range(8):
                    MM(t2[:, 0:512], xgT[p2][:, kc, :], wgu[wb][:, kc, :], kc == 0, kc == 7, [r_xgT[p2], r_wg[wb], r_wu[wb]], [r2])
                ACT(sge[p2][:, :], t2[:, 0:256], AF.Silu, [r2], [r_sge[p2]])
                TT("dve", ae[p2][:, :], sge[p2][:, :], t2[:, 256:512], ALU.mult, [r_sge[p2], r2], [r_ae[p2]])

            def stage_b(e_, blk, j):
                wb = e_ % NB
                p2 = j % 2
                t, r = PS()
                tb_ = t[:, :].bitcast(BF16)
                for c in range(2):
                    TR(tb_[:, c * 128:(c + 1) * 128], ae[p2][:, c * 128:(c + 1) * 128], identb[:, :], [r_ae[p2], r_identb], [r])
                ACT(aT[p2][:, :, :], tb_[:, 0:256].rearrange("p (a t) -> p a t", a=2), AF.Copy, [r], [r_aT[p2]])
                ty0, r_ty0 = PS()
                ty1, r_ty1 = PS()
                for kc in range(2):
                    MM(ty0[:, 0:512], aT[p2][:, kc, :], wd[wb][:, kc, 0:512], kc == 0, kc == 1, [r_aT[p2], r_wd[wb]], [r_ty0])
                    MM(ty1[:, 0:512], aT[p2][:, kc, :], wd[wb][:, kc, 512:1024], kc == 0, kc == 1, [r_aT[p2], r_wd[wb]], [r_ty1])
                py = j % NY
                nr = 128 if blk == 0 else CAP - 128
                ACT(ysb[py][0:nr, 0:512], ty0[0:nr, 0:512], AF.Copy, [r_ty0], [r_ysb[py]])
                CP("dve", ysb[py][0:nr, 512:1024], ty1[0:nr, 0:512], [r_ty1], [r_ysb[py]])
                ry = Res(f"ys{e_}_{blk}", semgrp=g_ys[py])
                r_ys_all.append(ry)
                s0 = 0 if blk == 0 else 128
                DMA("sp", ys_v[s0:s0 + nr, e_, :], ysb[py][0:nr, :], [r_ysb[py]], [ry])

            work = [(e_, blk) for e_ in range(n_exp) for blk in range(2)]
            for j in range(len(work) + 1):
                if j < len(work):
                    if work[j][1] == 0:
                        load_w(work[j][0])
                    stage_a(work[j][0], work[j][1], j)
                if j >= 1:
                    stage_b(work[j - 1][0], work[j - 1][1], j - 1)

            ln_eng[0] = "pool"
            acc = [sb(f"acc{i}", [128, 1024]) for i in range(2)]
            r_acc = [Res(f"acc{i}") for i in range(2)]
            yk = [sb(f"yk{i}", [128, 1024]) for i in range(4)]
            r_yk = [Res(f"yk{i}") for i in range(4)]
            lnt2, r_lnt2 = None, None
            ot = [sb(f"ot{i}", [128, 1024]) for i in range(2)]
            r_ot = [Res(f"ot{i}") for i in range(2)]
            st6b, r_st6b = sb("st6b", [128, 2, 6]), Res("st6b")
            mvb, r_mvb = sb("mvb", [128, 2]), Res("mvb")
            smb, r_smb = sb("smb", [128, 4]), Res("smb")
            finals = []
            gk = 0
            for it in range(NT):
                p2 = it % 2
                DMA("sp", acc[p2][:, :], zacc[it * 128:(it + 1) * 128, :], [], [r_acc[p2]])
                for k in range(8):
                    col = it * 8 + k
                    yb = gk % 4
                    gk += 1
                    S.op("pool", lambda e, yb=yb, col=col: e.indirect_dma_start(
                        out=yk[yb][:, :], out_offset=None, in_=yslots,
                        in_offset=bass.IndirectOffsetOnAxis(ap=slot8i[:, col:col + 1], axis=0),
                        bounds_check=BC(e), oob_is_err=False),
                        [r_slot8i] + r_ys_all, [r_yk[yb]], dma=True)
                    STT(acc[p2][:, :], yk[yb][:, :], w8[:, col:col + 1], acc[p2][:, :], ALU.mult, ALU.add,
                        [r_yk[yb], r_w8, r_acc[p2]], [r_acc[p2]])
                layer_norm(acc[p2], r_acc[p2], g2, r_g2, b2, r_b2, ot[p2], r_ot[p2], lnt2, r_lnt2, st6b, r_st6b, mvb, r_mvb, smb, r_smb)
                finals.append(DMA("sp", out[it * 128:(it + 1) * 128, :], ot[p2][:, :], [r_ot[p2]], [Res(f"out{it}", semgrp=g_out2[p2])]))
            S.wait_final("sp", finals)
            S.emit()
    return nc


def _consts(half):
    c = {}
    tp = np.arange(128)[:, None]
    t = np.arange(128)[None, :]
    mcur = np.zeros((128, 4, 128), np.float32)
    mprev = np.zeros((128, 4, 128), np.float32)
    mfirst = np.zeros((128, 4, 128), np.float32)
    for g, w in enumerate((2, 4, 8, 16)):
        band = ((tp <= t) & (tp > t - w)).astype(np.float32)
        mcur[:, g, :] = band / w - (tp == t)
        mprev[:, g, :] = (((tp - 128) <= t) & ((tp - 128) > t - w)).astype(np.float32) / w
        cnt = np.minimum(t + 1, w).astype(np.float32)
        mfirst[:, g, :] = band / cnt - (tp == t)
    c["c_mcur"] = mcur
    c["c_mprev"] = mprev
    c["c_mfirst"] = mfirst if half == 0 else mcur
    c["c_uinc"] = (tp <= t).astype(np.float32) * (-1.0 / 16.0)
    c["c_rgt"] = (tp > t).astype(np.float32) * (-1.0 / 16.0)
    c["c_mask4"] = np.repeat((tp <= t).astype(np.float32)[:, None, :], 4, axis=1)
    c["c_ident"] = np.eye(128, dtype=np.float32)
    c["c_ustrict"] = (tp < t).astype(np.float32)
    c["c_ones"] = np.ones((128, 128), np.float32)
    c["c_iota"] = np.broadcast_to(np.arange(256, dtype=np.float32)[None, :], (128, 256)).copy()
    tk = (np.arange(NT, dtype=np.int32)[None, :] * 128 + np.arange(128, dtype=np.int32)[:, None]).astype(np.int32)
    c["c_tokid"] = np.repeat(tk[:, :, None], 2, axis=2)
    c["c_tblinit"] = np.full((128, 512), PAD_IDX, np.int32)
    return {k: np.ascontiguousarray(v) for k, v in c.items()}


def _relayout_gu(wg, wu):
    E = wg.shape[0]
    o = np.empty((E, 128, 8, 512), np.float32)
    o[:, :, :, 0:256] = wg.reshape(E, 8, 128, 256).transpose(0, 2, 1, 3)
    o[:, :, :, 256:512] = wu.reshape(E, 8, 128, 256).transpose(0, 2, 1, 3)
    return o.reshape(E, 128, 8 * 512)


def _relayout_d(wd):
    E = wd.shape[0]
    return np.ascontiguousarray(wd.reshape(E, 2, 128, 1024).transpose(0, 2, 1, 3)).reshape(E, 128, 2 * 1024)


def make_in_maps(x, w_in, gla_gate_w2, gla_gate_b, gla_norm_w, pool_w_group, pool_scale, w_out, ln1_g, ln1_b,
                 router_w, router_bias, w_exp_gate, w_exp_up, w_exp_down, w_sh_gate, w_sh_up, w_sh_down, ln2_g, ln2_b):
    f = lambda a: np.ascontiguousarray(np.asarray(a, dtype=np.float32))
    x = f(x)
    w_in0 = f(w_in)[0]
    wgl = np.zeros((1024, 128), np.float32)
    wgl[:, 0:16] = w_in0[:, 2048:2064]
    w2p = np.zeros((128, 256), np.float32)
    w2p[0:16] = f(gla_gate_w2)[0]
    w2p[32] = f(gla_gate_b)[0]
    bc = lambda v, n: np.ascontiguousarray(np.broadcast_to(f(v)[0][None, :], (128, n)))
    shared = {
        "w_in": w_in0, "wgl": wgl, "w2p": w2p,
        "normw": f(gla_norm_w)[0].reshape(128, 1).copy(),
        "poolw": f(pool_w_group)[0],
        "pscale": np.ascontiguousarray(f(pool_scale)[0].reshape(4, 128).T),
        "w_out": f(w_out)[0],
        "ln1g": bc(ln1_g, 1024), "ln1b": bc(ln1_b, 1024), "ln2g": bc(ln2_g, 1024), "ln2b": bc(ln2_b, 1024),
        "router_w": f(router_w)[0], "rbias": bc(router_bias, 256),
        "weg": f(w_exp_gate)[0], "weu": f(w_exp_up)[0], "wed": f(w_exp_down)[0],
        "wsg": f(w_sh_gate)[0], "wsu": f(w_sh_up)[0], "wsd": f(w_sh_down)[0],
    }
    cst = [_consts(0), _consts(1)]
    in_maps = []
    for c in range(NCORES):
        b, half = c // 2, c % 2
        xT = np.zeros((1024, 2 * TL), np.float32)
        if half == 1:
            xT[:, 0:TL] = x[b, 0:TL].T
        xT[:, TL:] = x[b, half * TL:(half + 1) * TL].T
        m = dict(shared)
        m.update(cst[half])
        m["xT"] = xT
        m["xtok"] = np.ascontiguousarray(x[b, half * TL:(half + 1) * TL])
        in_maps.append(m)
    return in_maps


_NC_CACHE = {}


def kernel(**inputs):
    in_maps = make_in_maps(**inputs)
    if "full" not in _NC_CACHE:
        _NC_CACHE["full"] = build("full")
    nc = _NC_CACHE["full"]
    res = run_bass_kernel_spmd(nc, in_maps, core_ids=list(range(NCORES)))
    outs = [np.asarray(r["out"], dtype=np.float32) for r in res.results]
    full = np.zeros((4, 4096, 1024), np.float32)
    for c in range(NCORES):
        b, half = c // 2, c % 2
        full[b, half * TL:(half + 1) * TL] = outs[c]
    return full
```

```python
import numpy as np
from contextlib import ExitStack
import concourse.bass as bass
import concourse.mybir as mybir
from concourse.bass_utils import run_bass_kernel_spmd

F32 = mybir.dt.float32
BF16 = mybir.dt.bfloat16
I32 = mybir.dt.int32
AF = mybir.ActivationFunctionType
ALU = mybir.AluOpType
AX = mybir.AxisListType

NCORES = 8
TL = 2048
NT = TL // 128
NE = 256
CAP = 192
ALPHA = 2.0 ** 0.25
LN_EPS = 1e-5
RMS_EPS = 1e-5
BIG = 1.0e9
PAD_IDX = 1 << 30


class Res:
    __slots__ = ("name", "last_write", "reads", "semgrp", "sem", "cnt", "excl")

    def __init__(self, name, semgrp=None, excl=False):
        self.name = name
        self.excl = excl
        self.last_write = None
        self.reads = []
        self.semgrp = semgrp if semgrp is not None else self
        self.sem = None
        self.cnt = 0


class Sched:
    ENGS = ("pe", "act", "dve", "pool", "sp")

    def __init__(self, nc, stack):
        self.nc = nc
        self.stack = stack
        self.q = {e: [] for e in self.ENGS}
        self.cnt = {e: 0 for e in self.ENGS}
        self.sem = {e: stack.enter_context(nc.semaphore("prog_" + e)) for e in self.ENGS}
        self.waited = {e: {} for e in self.ENGS}
        self.groups = []

    def _dma_grp(self, res):
        g = res.semgrp
        if g.sem is None:
            g.sem = self.stack.enter_context(self.nc.semaphore("dma_" + g.name))
            self.groups.append(g)
        return g

    def op(self, eng, fn, reads=(), writes=(), dma=False):
        xr = [r for r in reads if r.excl and r not in writes]
        if xr:
            reads = [r for r in reads if not r.excl]
            writes = list(writes) + xr
        deps = set()
        for r in reads:
            if r.last_write is not None:
                deps.add(r.last_write)
        for w in writes:
            if w.last_write is not None:
                deps.add(w.last_write)
            deps.update(w.reads)
        if dma:
            g = self._dma_grp(writes[0])
            g.cnt += 16
            token = (g.sem, g.cnt)
        else:
            self.cnt[eng] += 1
            token = (self.sem[eng], self.cnt[eng])
        if eng == "pe":
            deps = {d for d in deps if d[0] is not self.sem["pe"]}
        self.q[eng].append((fn, deps, token, dma))
        for r in reads:
            r.reads.append(token)
        for w in writes:
            w.last_write = token
            w.reads = []
        return token

    def barrier(self):
        toks = [(self.sem[e], self.cnt[e]) for e in self.ENGS if self.cnt[e] > 0]
        toks += [(g.sem, g.cnt) for g in self.groups]
        for e in self.ENGS:
            self.q[e].append((None, set(toks), None, False))

    def wait_final(self, eng, tokens):
        self.q[eng].append((None, set(tokens), None, False))

    def emit(self):
        nc = self.nc
        with nc.Block() as block:
            def run(engname, eng):
                waited = self.waited[engname]
                for (fn, deps, token, dma) in self.q[engname]:
                    best = {}
                    for (s, v) in deps:
                        if v > best.get(s.num, (None, 0))[1]:
                            best[s.num] = (s, v)
                    for k, (s, v) in best.items():
                        if waited.get(k, 0) >= v:
                            continue
                        eng.wait_ge(s, v)
                        waited[k] = v
                    if fn is None:
                        continue
                    ins = fn(eng)
                    ins.then_inc(token[0], 16 if dma else 1)
                self.q[engname] = []

            @block.tensor
            def _(e):
                run("pe", e)

            @block.scalar
            def _(e):
                run("act", e)

            @block.vector
            def _(e):
                run("dve", e)

            @block.gpsimd
            def _(e):
                run("pool", e)

            @block.sync
            def _(e):
                run("sp", e)


def build(stage="full", n_exp=NE):
    nc = bass.Bass("TRN2", target_bir_lowering=False)

    def din(name, shape, dt=F32):
        return nc.dram_tensor(name, list(shape), dt, kind="ExternalInput").ap()

    xT = din("xT", [1024, 2 * TL])
    xtok = din("xtok", [TL, 1024])
    w_in = din("w_in", [1024, 2064])
    wgl = din("wgl", [1024, 128])
    w2p = din("w2p", [128, 256])
    normw = din("normw", [128, 1])
    poolw = din("poolw", [4, 128, 128])
    pscale = din("pscale", [128, 4])
    w_out = din("w_out", [1024, 1024])
    ln1g = din("ln1g", [128, 1024])
    ln1b = din("ln1b", [128, 1024])
    ln2g = din("ln2g", [128, 1024])
    ln2b = din("ln2b", [128, 1024])
    router_w = din("router_w", [1024, 256])
    rbias = din("rbias", [128, 256])
    if stage == "full":
        weg = din("weg", [NE, 1024, 256])
        weu = din("weu", [NE, 1024, 256])
        wed = din("wed", [NE, 256, 1024])
    if stage == "route":
        dbg_slot = nc.dram_tensor("dbg_slot", [128, NT * 8], F32, kind="ExternalOutput").ap()
        dbg_w8 = nc.dram_tensor("dbg_w8", [128, NT * 8], F32, kind="ExternalOutput").ap()
        dbg_tbl = nc.dram_tensor("dbg_tbl", [128, 2 * NE], I32, kind="ExternalOutput").ap()
    wsg = din("wsg", [1024, 256])
    wsu = din("wsu", [1024, 256])
    wsd = din("wsd", [256, 1024])
    c_mcur = din("c_mcur", [128, 4, 128])
    c_mprev = din("c_mprev", [128, 4, 128])
    c_mfirst = din("c_mfirst", [128, 4, 128])
    c_uinc = din("c_uinc", [128, 128])
    c_rgt = din("c_rgt", [128, 128])
    c_mask4 = din("c_mask4", [128, 4, 128])
    c_ident = din("c_ident", [128, 128])
    c_ustrict = din("c_ustrict", [128, 128])
    c_ones = din("c_ones", [128, 128])
    c_iota = din("c_iota", [128, 256])
    c_tokid = din("c_tokid", [128, NT, 2], I32)
    c_tblinit = din("c_tblinit", [128, 512], I32)
    out = nc.dram_tensor("out", [TL, 1024], F32, kind="ExternalOutput").ap()

    hbf = nc.dram_tensor("hbf", [TL + 1, 1024], BF16).ap()
    zacc = nc.dram_tensor("zacc", [TL, 1024], F32).ap()
    slot_tok = nc.dram_tensor("slot_tok", [CAP * NE, 2], I32).ap()
    yslots = nc.dram_tensor("yslots", [CAP * NE, 1024], F32).ap()

    with ExitStack() as st0:
        S = Sched(nc, st0)

        def mk(st):
            def sb(name, shape, dt=F32):
                return st.enter_context(nc.sbuf_tensor(name, list(shape), dt))
            return sb

        sb0 = mk(st0)

        def MM(o, lhsT, rhs, start, stop, r, w):
            S.op("pe", lambda e: e.matmul(o, lhsT, rhs, start=start, stop=stop), r, w)

        def TR(o, in_, ident, r, w):
            S.op("pe", lambda e: e.transpose(o, in_, ident), r, w)

        def ACT(o, in_, func, r, w, **kw):
            return S.op("act", lambda e: e.activation(out=o, in_=in_, func=func, **kw), r, w)

        def TT(eng, o, a, b, op, r, w):
            return S.op(eng, lambda e: e.tensor_tensor(out=o, in0=a, in1=b, op=op), r, w)

        def TS(eng, o, a, s1, s2, op0, op1, r, w, **kw):
            if op1 is None:
                return S.op(eng, lambda e: e.tensor_scalar(out=o, in0=a, scalar1=s1, scalar2=None, op0=op0, **kw), r, w)
            return S.op(eng, lambda e: e.tensor_scalar(out=o, in0=a, scalar1=s1, scalar2=s2, op0=op0, op1=op1, **kw), r, w)

        def STT(o, a, s, b, op0, op1, r, w, **kw):
            return S.op("dve", lambda e: e.scalar_tensor_tensor(out=o, in0=a, scalar=s, in1=b, op0=op0, op1=op1, **kw), r, w)

        def CP(eng, o, a, r, w):
            return S.op(eng, lambda e: e.tensor_copy(out=o, in_=a), r, w)

        def DMA(q, o, in_, r, w, **kw):
            return S.op(q, lambda e: e.dma_start(out=o, in_=in_, **kw), r, w, dma=True)

        bc_reg = {}

        def BCG(e):
            if "g" not in bc_reg:
                bc_reg["g"] = e.alloc_register("bcg")
                e.reg_mov(bc_reg["g"], TL - 1)
            return bc_reg["g"]

        def BC(e):
            if "r" not in bc_reg:
                bc_reg["r"] = e.alloc_register("bc")
                e.reg_mov(bc_reg["r"], CAP * NE - 1)
            return bc_reg["r"]

        banks = []
        for i in range(8):
            t = st0.enter_context(nc.psum_tensor(f"bank{i}", [128, 512], F32))
            banks.append((t, Res(f"bank{i}", excl=True)))
        bk = [0]

        def PS():
            b = banks[bk[0] % 8]
            bk[0] += 1
            return b

        def const_load(name, src, shape, dt=F32, q="sp"):
            t = sb0(name, shape, dt)
            r = Res(name)
            DMA(q, t[:], src, [], [r])
            return t, r

        ident32, r_ident32 = const_load("ident32", c_ident, [128, 128])
        identb, r_identb = const_load("identb", c_ident, [128, 128], BF16, "pool")
        tokid, r_tokid = const_load("tokid", c_tokid, [128, NT, 2], I32)
        slot8f = sb0("slot8f", [128, NT * 8]); r_slot8f = Res("slot8f")
        slot8i = sb0("slot8i", [128, NT * 8], I32); r_slot8i = Res("slot8i")
        w8 = sb0("w8", [128, NT * 8]); r_w8 = Res("w8")
        g_out = Res("outgrp")
        g_out2 = [Res("outgrp0"), Res("outgrp1")]
        g_hbf = Res("hbf")
        g_zacc = Res("zacc")
        g_slot = Res("slot_tok")
        r_slotinit = Res("slot_init")

        ln_eng = ["dve"]

        def layer_norm(z, r_z, gam, r_gam, bet, r_bet, o, r_o, tmp, r_tmp, st6, r_st6, mv, r_mv, sm, r_sm):
            S.op("dve", lambda e: e.bn_stats(out=st6[:, 0, :], in_=z[:, 0:512]), [r_z], [r_st6])
            S.op("dve", lambda e: e.bn_stats(out=st6[:, 1, :], in_=z[:, 512:1024]), [r_z], [r_st6])
            S.op("dve", lambda e: e.bn_aggr(out=mv[:, :], in_=st6[:, :, :]), [r_st6], [r_mv])
            ACT(sm[:, 0:1], mv[:, 1:2], AF.Ln, [r_mv], [r_sm], bias=LN_EPS)
            ACT(sm[:, 1:2], sm[:, 0:1], AF.Exp, [r_sm], [r_sm], scale=-0.5)
            TS("dve", sm[:, 2:3], mv[:, 0:1], sm[:, 1:2], -1.0, ALU.mult, ALU.mult, [r_mv, r_sm], [r_sm])
            ACT(o[:, :], z[:, :], AF.Identity, [r_z, r_sm], [r_o], scale=sm[:, 1:2], bias=sm[:, 2:3])
            TT(ln_eng[0], o[:, :], o[:, :], gam[:, :], ALU.mult, [r_o, r_gam], [r_o])
            TT(ln_eng[0], o[:, :], o[:, :], bet[:, :], ALU.add, [r_o, r_bet], [r_o])

        with ExitStack() as st1:
            sb = mk(st1)
            w_in_bf = sb("w_in_bf", [128, 8, 2064], BF16)
            r_win = [Res("win_a"), Res("win_b")]
            w_in_v = w_in.rearrange("(c p) n -> p c n", p=128)
            DMA("pool", w_in_bf[:, :, 0:1024], w_in_v[:, :, 0:1024], [], [r_win[0]])
            DMA("pool", w_in_bf[:, :, 1024:2064], w_in_v[:, :, 1024:2064], [], [r_win[1]])
            wgl_bf = sb("wgl_bf", [128, 8, 128], BF16); r_wgl = Res("wgl")
            DMA("pool", wgl_bf[:], wgl.rearrange("(c p) n -> p c n", p=128), [], [r_wgl])
            mcur, r_mcur = sb("mcur", [128, 4, 128], BF16), Res("mcur")
            DMA("pool", mcur[:], c_mcur, [], [r_mcur])
            mprev, r_mprev = sb("mprev", [128, 4, 128], BF16), Res("mprev")
            DMA("pool", mprev[:], c_mprev, [], [r_mprev])
            mfirst, r_mfirst = sb("mfirst", [128, 4, 128], BF16), Res("mfirst")
            DMA("pool", mfirst[:], c_mfirst, [], [r_mfirst])
            poolw_bf, r_poolw = sb("poolw_bf", [128, 4, 128], BF16), Res("poolw")
            DMA("pool", poolw_bf[:], poolw.rearrange("g c d -> c g d"), [], [r_poolw])
            w_out_bf, r_wout = sb("w_out_bf", [128, 8, 1024], BF16), Res("wout")
            DMA("pool", w_out_bf[:], w_out.rearrange("(c p) n -> p c n", p=128), [], [r_wout])
            wsgu, r_wsgu = sb("wsgu", [128, 8, 512], BF16), [Res("wsg"), Res("wsu")]
            DMA("pool", wsgu[:, :, 0:256], wsg.rearrange("(c p) n -> p c n", p=128), [], [r_wsgu[0]])
            DMA("pool", wsgu[:, :, 256:512], wsu.rearrange("(c p) n -> p c n", p=128), [], [r_wsgu[1]])
            wsd_bf, r_wsd = sb("wsd_bf", [128, 2, 1024], BF16), Res("wsd")
            DMA("pool", wsd_bf[:], wsd.rearrange("(c p) n -> p c n", p=128), [], [r_wsd])
            rw_sb, r_rw = sb("rw_sb", [128, 8, 256]), Res("rw")
            DMA("sp", rw_sb[:], router_w.rearrange("(c p) n -> p c n", p=128), [], [r_rw])
            uinc, r_uinc = sb("uinc", [128, 128]), Res("uinc")
            DMA("sp", uinc[:], c_uinc, [], [r_uinc])
            rgt, r_rgt = sb("rgt", [128, 128]), Res("rgt")
            DMA("sp", rgt[:], c_rgt, [], [r_rgt])
            mask4, r_mask4 = sb("mask4", [128, 4, 128], BF16), Res("mask4")
            DMA("pool", mask4[:], c_mask4, [], [r_mask4])
            ustrict, r_ustrict = sb("ustrict", [128, 128]), Res("ustrict")
            DMA("sp", ustrict[:], c_ustrict, [], [r_ustrict])
            ones32, r_ones = sb("ones32", [128, 128]), Res("ones")
            DMA("sp", ones32[:], c_ones, [], [r_ones])
            iota_e, r_iota = sb("iota_e", [128, 256]), Res("iota")
            DMA("sp", iota_e[:], c_iota, [], [r_iota])
            w2p_sb, r_w2p = sb("w2p_sb", [128, 256]), Res("w2p")
            DMA("sp", w2p_sb[:], w2p, [], [r_w2p])
            normw_sb, r_normw = sb("normw_sb", [128, 1]), Res("normw")
            DMA("sp", normw_sb[:], normw, [], [r_normw])
            pscale_sb, r_pscale = sb("pscale_sb", [128, 4]), Res("pscale")
            DMA("sp", pscale_sb[:], pscale, [], [r_pscale])
            rbias_sb, r_rbias = sb("rbias_sb", [128, 256]), Res("rbias")
            DMA("sp", rbias_sb[:], rbias, [], [r_rbias])
            g1, r_g1 = sb("g1", [128, 1024]), Res("g1")
            DMA("sp", g1[:], ln1g, [], [r_g1])
            b1, r_b1 = sb("b1", [128, 1024]), Res("b1")
            DMA("sp", b1[:], ln1b, [], [r_b1])
            tblinit, r_tblinit = sb("tblinit", [128, 512], I32), Res("tblinit")
            DMA("sp", tblinit[:], c_tblinit, [], [r_tblinit])
            st_v = slot_tok.rearrange("(s e) o -> s (e o)", e=NE)
            DMA("sp", st_v[0:128, :], tblinit[:], [r_tblinit], [r_slotinit])
            r_slotinit_b = Res("slot_init_b")
            DMA("sp", st_v[128:CAP, :], tblinit[0:CAP - 128, :], [r_tblinit], [r_slotinit_b])

            xTb = [sb(f"xTb{i}", [128, 8, 512], BF16) for i in range(2)]
            r_xTb = [Res(f"xTb{i}") for i in range(2)]
            glT, r_glT = sb("glT", [128, 512]), Res("glT")
            S.op("dve", lambda e: e.memset(glT[:], 0.0), [], [r_glT])
            S.op("dve", lambda e: e.memset(glT[32:33, :], 1.0), [], [r_glT])
            DMA("sp", hbf[TL:TL + 1, :], glT[64:65, :].bitcast(BF16), [r_glT], [Res("hbf_zero", semgrp=g_hbf)])
            vtok = [sb(f"vtok{i}", [128, 512], BF16) for i in range(2)]
            r_vtok = [Res(f"vtok{i}") for i in range(2)]
            ptok = [sb(f"ptok{i}", [128, 512], BF16) for i in range(2)]
            r_ptok = [Res(f"ptok{i}") for i in range(2)]
            S.op("dve", lambda e: e.memset(ptok[0][:], 0.0), [], [r_ptok[0]])
            S.op("dve", lambda e: e.memset(ptok[1][:], 0.0), [], [r_ptok[1]])
            ez, r_ez = sb("ez", [128, 256]), Res("ez")
            sp_, r_sp = sb("sp_", [128, 256]), Res("sp_")
            eb, r_eb = sb("eb", [128, 256]), Res("eb")
            kdec, r_kdec = sb("kdec", [128, 256], BF16), Res("kdec")
            dec, r_dec = sb("dec", [128, 4]), Res("dec")
            S32, r_S32 = sb("S32", [128, 2, 256]), Res("S32")
            S.op("dve", lambda e: e.memset(S32[:], 0.0), [], [r_S32])
            Sbf, r_Sbf = sb("Sbf", [128, 2, 256], BF16), Res("Sbf")
            qT32, r_qT32 = sb("qT32", [128, 2, 512]), Res("qT32")
            kT32, r_kT32 = sb("kT32", [128, 2, 512]), Res("kT32")
            srT, r_srT = sb("srT", [128, 4, 512]), Res("srT")
            ebT, r_ebT = sb("ebT", [128, 256]), Res("ebT")
            enbT, r_enbT = sb("enbT", [128, 256]), Res("enbT")
            ktT, r_ktT = sb("ktT", [128, 2, 128], BF16), Res("ktT")
            qTz = [sb(f"qTz{i}", [128, 4, 128], BF16) for i in range(2)]
            r_qTz = [Res(f"qTz{i}") for i in range(2)]
            S.op("dve", lambda e: e.memset(qTz[0][:], 0.0), [], [r_qTz[0]])
            S.op("dve", lambda e: e.memset(qTz[1][:], 0.0), [], [r_qTz[1]])
            sTm, r_sTm = sb("sTm", [128, 4, 128], BF16), Res("sTm")
            osq, r_osq = sb("osq", [128, 512]), Res("osq")
            lnv, r_lnv = osq, r_osq
            rstd, r_rstd = osq, r_osq
            on_, r_on = sb("on_", [128, 512]), Res("on_")
            mixT, r_mixT = sb("mixT", [128, 4, 128], BF16), Res("mixT")
            catT, r_catT = sb("catT", [128, 8, 128], BF16), Res("catT")
            xt0 = sb("xt0", [128, 1024])
            xt = [xt0, xt0]
            r_xt0 = Res("xt0")
            r_xt = [r_xt0, r_xt0]
            z1, r_z1 = sb("z1", [128, 1024]), Res("z1")
            lnt, r_lnt = None, None
            st6, r_st6 = sb("st6", [128, 2, 6]), Res("st6")
            mv, r_mv = sb("mv", [128, 2]), Res("mv")
            sm, r_sm = sb("sm", [128, 4]), Res("sm")
            hring = [sb(f"h{i}", [128, 1024]) for i in range(4)]
            r_hring = [Res(f"h{i}") for i in range(4)]
            hb, r_hb = sb("hb", [128, 1024], BF16), Res("hb")
            hT32, r_hT32 = sb("hT32", [128, 8, 128]), Res("hT32")
            hTb, r_hTb = sb("hTb", [128, 8, 512], BF16), Res("hTb")
            scores2 = [sb(f"scores{i}", [128, 256]) for i in range(2)]
            r_scores2 = [Res(f"scores{i}") for i in range(2)]
            pending = [None]
            prev = [None, None, None]
            v8s, r_v8s = sb("v8s", [128, 8]), Res("v8s")
            biased, r_biased = sb("biased", [128, 256]), Res("biased")
            tmpA, r_tmpA = sb("tmpA", [128, 256]), Res("tmpA")
            tmpB, r_tmpB = sb("tmpB", [128, 256]), Res("tmpB")
            g8, r_g8 = sb("g8", [128, 32]), Res("g8")
            v8, r_v8 = sb("v8", [128, 8]), Res("v8")
            mbv, r_mbv = sb("mbv", [128, 256]), Res("mbv")
            sel, r_sel = sb("sel", [128, 256]), Res("sel")
            selcum, r_selcum = sb("selcum", [128, 256]), Res("selcum")
            S.op("dve", lambda e: e.memset(selcum[:], 0.0), [], [r_selcum])
            gs, r_gs = tmpB, r_tmpB
            rs, r_rs = sb("rs", [128, 2]), Res("rs")
            slotfull, r_slotfull = sb("slotfull", [128, 256]), Res("slotfull")
            junk, r_junk = tmpA, r_tmpA
            sg, r_sg = sb("sg", [128, 512]), Res("sg")
            actsh, r_actsh = sb("actsh", [128, 2, 512], BF16), Res("actsh")
            za0 = sb("za0", [128, 1024])
            za = [za0, za0]
            r_za0 = Res("za0")
            r_za = [r_za0, r_za0]

            xT_v = xT.rearrange("(c p) t -> p c t", p=128)

            def load_x_block(gblk):
                i = gblk % 2
                DMA("pool", xTb[i][:], xT_v[:, :, gblk * 512:(gblk + 1) * 512], [], [r_xTb[i]])
                return xTb[i], r_xTb[i]

            def proj_glow(xb, r_xb):
                t, r = PS()
                for kc in range(8):
                    MM(t[:, 0:512], wgl_bf[:, kc, :], xb[:, kc, :], kc == 0, kc == 7, [r_wgl, r_xb], [r])
                ACT(glT[0:32, :], t[0:32, 0:512], AF.Copy, [r], [r_glT])

            def tok_proj(xb, r_xb, cols, c0, c1, n):
                t, r = PS()
                for kc in range(8):
                    MM(t[:, 0:n], xb[:, kc, cols], w_in_bf[:, kc, c0:c1], kc == 0, kc == 7, [r_xb] + r_win, [r])
                return t, r

            def gate_and_kdec(cols, tk, r_tk):
                t, r = PS()
                MM(t[:, 0:256], glT[:, cols], w2p_sb[:, :], True, True, [r_glT, r_w2p], [r])
                ACT(ez[:, :], t[:, 0:256], AF.Exp, [r], [r_ez], scale=-1.0)
                ACT(sp_[:, :], ez[:, :], AF.Ln, [r_ez], [r_sp], bias=1.0)
                t2, r2 = PS()
                MM(t2[:, 0:256], rgt[:, :], sp_[:, :], True, True, [r_rgt, r_sp], [r2])
                ACT(eb[:, :], t2[:, 0:256], AF.Exp, [r2], [r_eb])
                TT("dve", kdec[:, :], tk[:, 0:256], eb[:, :], ALU.mult, [r_tk, r_eb], [r_kdec])

            def state_update(par, dec_ap_fn, r_decsrc):
                t, r = PS()
                for half in range(2):
                    MM(t[:, half * 256:(half + 1) * 256], kdec[:, half * 128:(half + 1) * 128],
                       vtok[par][:, half * 256:(half + 1) * 256], True, True, [r_kdec, r_vtok[par]], [r])
                for half in range(2):
                    STT(S32[:, half, :], S32[:, half, :], dec_ap_fn(half), t[:, half * 256:(half + 1) * 256],
                        ALU.mult, ALU.add, [r_S32, r_decsrc, r], [r_S32])

            gtile = 0
            for blk in range(4):
                xb, r_xb = load_x_block(blk)
                proj_glow(xb, r_xb)
                for t4 in range(4):
                    it = blk * 4 + t4
                    par = gtile % 2
                    gtile += 1
                    cols = slice(t4 * 128, (t4 + 1) * 128)
                    tv, r_tv = tok_proj(xb, r_xb, cols, 1024, 1536, 512)
                    tk, r_tk = tok_proj(xb, r_xb, cols, 768, 1024, 256)
                    ACT(vtok[par][:, :], tv[:, 0:512], AF.Copy, [r_tv], [r_vtok[par]])
                    gate_and_kdec(cols, tk, r_tk)
                    tb, r_tb = PS()
                    for half in range(2):
                        MM(tb[:, 2 * half:2 * half + 2], sp_[:, half * 128:(half + 1) * 128], uinc[:, 126:128],
                           True, True, [r_sp, r_uinc], [r_tb])
                    ACT(dec[:, :], tb[:, 0:4], AF.Exp, [r_tb], [r_dec])
                    state_update(par, lambda half: dec[:, 2 * half + 1:2 * half + 2], r_dec)
                    if it == 15:
                        tp, r_tp = tok_proj(xb, r_xb, cols, 0, 512, 512)
                        ACT(ptok[par][:, :], tp[:, 0:512], AF.Copy, [r_tp], [r_ptok[par]])
            ACT(Sbf[:, :, :], S32[:, :, :], AF.Copy, [r_S32], [r_Sbf])

            nxt = load_x_block(4)
            for blk in range(4):
                xb, r_xb = nxt
                if blk < 3:
                    nxt = load_x_block(4 + blk + 1)
                proj_glow(xb, r_xb)
                for m in range(2):
                    t, r = PS()
                    for kc in range(8):
                        MM(t[:, 0:512], w_in_bf[:, kc, 512 + m * 128:512 + (m + 1) * 128], xb[:, kc, :], kc == 0, kc == 7,
                           r_win + [r_xb], [r])
                    ACT(qT32[:, m, :], t[:, 0:512], AF.Copy, [r], [r_qT32])
                for m in range(2):
                    t, r = PS()
                    for kc in range(8):
                        MM(t[:, 0:512], w_in_bf[:, kc, 768 + m * 128:768 + (m + 1) * 128], xb[:, kc, :], kc == 0, kc == 7,
                           r_win + [r_xb], [r])
                    CP("dve", kT32[:, m, :], t[:, 0:512], [r], [r_kT32])
                for m in range(4):
                    t, r = PS()
                    for kc in range(8):
                        MM(t[:, 0:512], w_in_bf[:, kc, 1536 + m * 128:1536 + (m + 1) * 128], xb[:, kc, :], kc == 0, kc == 7,
                           r_win + [r_xb], [r])
                    ACT(srT[:, m, :], t[:, 0:512], AF.Silu, [r], [r_srT])

                for t4 in range(4):
                    it = blk * 4 + t4
                    par = gtile % 2
                    gtile += 1
                    cols = slice(t4 * 128, (t4 + 1) * 128)
                    hcur, r_hcur = hring[it % 4], r_hring[it % 4]
                    DMA("sp", xt[par][:, :], xtok[it * 128:(it + 1) * 128, :], [], [r_xt[par]])
                    tp, r_tp = tok_proj(xb, r_xb, cols, 0, 512, 512)
                    tv, r_tv = tok_proj(xb, r_xb, cols, 1024, 1536, 512)
                    tk, r_tk = tok_proj(xb, r_xb, cols, 768, 1024, 256)
                    ACT(ptok[par][:, :], tp[:, 0:512], AF.Copy, [r_tp], [r_ptok[par]])
                    ACT(vtok[par][:, :], tv[:, 0:512], AF.Copy, [r_tv], [r_vtok[par]])
                    gate_and_kdec(cols, tk, r_tk)
                    tb, r_tb = PS()
                    for half in range(2):
                        MM(tb[:, half * 128:(half + 1) * 128], sp_[:, half * 128:(half + 1) * 128], uinc[:, :],
                           True, True, [r_sp, r_uinc], [r_tb])
                    ACT(ebT[:, :], tb[:, 0:256], AF.Exp, [r_tb], [r_ebT])
                    ACT(enbT[:, :], tb[:, 0:256], AF.Exp, [r_tb], [r_enbT], scale=-1.0)
                    TT("dve", ktT[:, :, :], kT32[:, :, cols], enbT[:, :].rearrange("p (a t) -> p a t", a=2), ALU.mult,
                       [r_kT32, r_enbT], [r_ktT])
                    for h in range(4):
                        r0 = (h % 2) * 64
                        half = h // 2
                        STT(qTz[par][r0:r0 + 64, h, :], qT32[r0:r0 + 64, half, cols], 0.125,
                            ebT[r0:r0 + 64, half * 128:(half + 1) * 128], ALU.mult, ALU.mult,
                            [r_qT32, r_ebT], [r_qTz[par]])
                    ts_, r_ts = PS()
                    for h in range(4):
                        MM(ts_[:, h * 128:(h + 1) * 128], ktT[:, h // 2, :], qTz[par][:, h, :], True, True,
                           [r_ktT, r_qTz[par]], [r_ts])
                    TT("dve", sTm[:, :, :], ts_[:, 0:512].rearrange("p (a t) -> p a t", a=4), mask4[:, :, :], ALU.mult,
                       [r_ts, r_mask4], [r_sTm])
                    to, r_to = PS()
                    for h in range(4):
                        MM(to[:, h * 128:(h + 1) * 128], vtok[par][:, h * 128:(h + 1) * 128], sTm[:, h, :], True, False,
                           [r_vtok[par], r_sTm], [r_to])
                        MM(to[:, h * 128:(h + 1) * 128], Sbf[:, h // 2, (h % 2) * 128:(h % 2 + 1) * 128], qTz[par][:, h, :],
                           False, True, [r_Sbf, r_qTz[par]], [r_to])
                    ACT(osq[:, :], to[:, 0:512], AF.Square, [r_to], [r_osq])
                    tss, r_tss = PS()
                    MM(tss[:, 0:512], ones32[:, :], osq[:, :], True, True, [r_ones, r_osq], [r_tss])
                    ACT(lnv[:, :], tss[:, 0:512], AF.Ln, [r_tss], [r_lnv], scale=1.0 / 128.0, bias=RMS_EPS)
                    ACT(rstd[:, :], lnv[:, :], AF.Exp, [r_lnv], [r_rstd], scale=-0.5)
                    TT("dve", on_[:, :], to[:, 0:512], rstd[:, :], ALU.mult, [r_to, r_rstd], [r_on])
                    STT(catT[:, 4:8, :], on_[:, :].rearrange("p (a t) -> p a t", a=4), normw_sb[:, 0:1], srT[:, :, cols],
                        ALU.mult, ALU.mult, [r_on, r_normw, r_srT], [r_catT])
                    tpm, r_tpm = PS()
                    mc, r_mc = (mfirst, r_mfirst) if it == 0 else (mcur, r_mcur)
                    for g in range(4):
                        MM(tpm[:, g * 128:(g + 1) * 128], ptok[par][:, g * 128:(g + 1) * 128], mc[:, g, :], True, False,
                           [r_ptok[par], r_mc], [r_tpm])
                        MM(tpm[:, g * 128:(g + 1) * 128], ptok[1 - par][:, g * 128:(g + 1) * 128], mprev[:, g, :], False, True,
                           [r_ptok[1 - par], r_mprev], [r_tpm])
                    ACT(mixT[:, :, :], tpm[:, 0:512].rearrange("p (a t) -> p a t", a=4), AF.Copy, [r_tpm], [r_mixT])
                    tpo, r_tpo = PS()
                    for g in range(4):
                        MM(tpo[:, g * 128:(g + 1) * 128], poolw_bf[:, g, :], mixT[:, g, :], True, True, [r_poolw, r_mixT], [r_tpo])
                    for g in range(4):
                        TS("dve", catT[:, g, :], tpo[:, g * 128:(g + 1) * 128], pscale_sb[:, g:g + 1], None, ALU.mult, None,
                           [r_tpo, r_pscale], [r_catT])
                    state_update(par, lambda half: ebT[:, half * 128 + 127:half * 128 + 128], r_ebT)
                    ACT(Sbf[:, :, :], S32[:, :, :], AF.Copy, [r_S32], [r_Sbf])
                    def part2(it=it, par=par, hcur=hcur, r_hcur=r_hcur):
                        tm0, r_tm0 = PS()
                        tm1, r_tm1 = PS()
                        for kc in range(8):
                            MM(tm0[:, 0:512], catT[:, kc, :], w_out_bf[:, kc, 0:512], kc == 0, kc == 7, [r_catT, r_wout], [r_tm0])
                            MM(tm1[:, 0:512], catT[:, kc, :], w_out_bf[:, kc, 512:1024], kc == 0, kc == 7, [r_catT, r_wout], [r_tm1])
                        STT(z1[:, 0:512], xt[par][:, 0:512], ALPHA, tm0[:, 0:512], ALU.mult, ALU.add, [r_xt[par], r_tm0], [r_z1])
                        STT(z1[:, 512:1024], xt[par][:, 512:1024], ALPHA, tm1[:, 0:512], ALU.mult, ALU.add, [r_xt[par], r_tm1], [r_z1])
                        layer_norm(z1, r_z1, g1, r_g1, b1, r_b1, hcur, r_hcur, lnt, r_lnt, st6, r_st6, mv, r_mv, sm, r_sm)
                    def route_a(it=it, cols=cols, hcur=hcur, r_hcur=r_hcur):
                        ACT(hb[:, :], hcur[:, :], AF.Copy, [r_hcur], [r_hb])
                        DMA("sp", hbf[it * 128:(it + 1) * 128, :], hb[:, :], [r_hb], [Res(f"hbf{it}", semgrp=g_hbf)])
                        tt0, r_tt0 = PS()
                        tt1, r_tt1 = PS()
                        for c in range(8):
                            tt, r_tt = (tt0, r_tt0) if c < 4 else (tt1, r_tt1)
                            TR(tt[:, (c % 4) * 128:(c % 4 + 1) * 128], hcur[:, c * 128:(c + 1) * 128], ident32[:, :],
                               [r_hcur, r_ident32], [r_tt])
                        CP("dve", hT32[:, 0:4, :], tt0[:, 0:512].rearrange("p (a t) -> p a t", a=4), [r_tt0], [r_hT32])
                        CP("dve", hT32[:, 4:8, :], tt1[:, 0:512].rearrange("p (a t) -> p a t", a=4), [r_tt1], [r_hT32])
                        ACT(hTb[:, 0:4, cols], tt0[:, 0:512].rearrange("p (a t) -> p a t", a=4), AF.Copy, [r_tt0], [r_hTb])
                        ACT(hTb[:, 4:8, cols], tt1[:, 0:512].rearrange("p (a t) -> p a t", a=4), AF.Copy, [r_tt1], [r_hTb])
                        trt, r_trt = PS()
                        for kc in range(8):
                            MM(trt[:, 0:256], hT32[:, kc, :], rw_sb[:, kc, :], kc == 0, kc == 7, [r_hT32, r_rw], [r_trt])
                        scores, r_scores = scores2[it % 2], r_scores2[it % 2]
                        ACT(scores[:, :], trt[:, 0:256], AF.Sigmoid, [r_trt], [r_scores])
                    def route_b(it=it):
                        scores, r_scores = scores2[it % 2], r_scores2[it % 2]
                        TT("dve", biased[:, :], scores[:, :], rbias_sb[:, :], ALU.add, [r_scores, r_rbias], [r_biased])
                        b3 = biased[:, :].rearrange("p (g i) -> p g i", g=8)
                        S.op("dve", lambda e, b3=b3: e.tensor_reduce(out=g8[:, 0:8], in_=b3, axis=AX.X, op=ALU.max), [r_biased], [r_g8])
                        TT("dve", tmpA[:, :].rearrange("p (g i) -> p g i", g=8), b3, g8[:, 0:8].unsqueeze(2).to_broadcast([128, 8, 32]),
                           ALU.is_equal, [r_biased, r_g8], [r_tmpA])
                        STT(tmpB[:, :], tmpA[:, :], -BIG, biased[:, :], ALU.mult, ALU.add, [r_tmpA, r_biased], [r_tmpB])
                        tb3 = tmpB[:, :].rearrange("p (g i) -> p g i", g=8)
                        S.op("dve", lambda e, tb3=tb3: e.tensor_reduce(out=g8[:, 8:16], in_=tb3, axis=AX.X, op=ALU.max), [r_tmpB], [r_g8])
                        TT("dve", g8[:, 8:16], g8[:, 8:16], g8[:, 0:8], ALU.add, [r_g8], [r_g8])
                        S.op("dve", lambda e: e.max(out=g8[:, 16:24], in_=g8[:, 8:16]), [r_g8], [r_g8])
                        TS("dve", g8[:, 24:32], g8[:, 8:16], g8[:, 19:20], None, ALU.is_ge, None, [r_g8], [r_g8])
                        TS("dve", g8[:, 24:32], g8[:, 24:32], -1.0, BIG, ALU.add, ALU.mult, [r_g8], [r_g8])
                        TT("dve", mbv[:, :].rearrange("p (g i) -> p g i", g=8), b3, g8[:, 24:32].unsqueeze(2).to_broadcast([128, 8, 32]),
                           ALU.add, [r_biased, r_g8], [r_mbv])
                        S.op("dve", lambda e: e.max(out=v8[:, :], in_=mbv[:, :]), [r_mbv], [r_v8])
                        TS("dve", sel[:, :], mbv[:, :], v8[:, 7:8], None, ALU.is_ge, None, [r_mbv, r_v8], [r_sel])
                        TT("dve", gs[:, :], sel[:, :], scores[:, :], ALU.mult, [r_sel, r_scores], [r_gs])
                        S.op("dve", lambda e: e.max(out=v8s[:, :], in_=gs[:, :]), [r_gs], [r_v8s])
                        S.op("dve", lambda e: e.tensor_reduce(out=rs[:, 0:1], in_=v8s[:, :], axis=AX.X, op=ALU.add), [r_v8s], [r_rs])
                        S.op("dve", lambda e: e.reciprocal(out=rs[:, 1:2], in_=rs[:, 0:1]), [r_rs], [r_rs])
                        TS("dve", w8[:, it * 8:(it + 1) * 8], v8s[:, :], rs[:, 1:2], 2.5, ALU.mult, ALU.mult, [r_v8s, r_rs], [r_w8])
                        tps, r_tps = PS()
                        MM(tps[:, 0:256], ones32[:, :], selcum[:, :], True, False, [r_ones, r_selcum], [r_tps])
                        MM(tps[:, 0:256], ustrict[:, :], sel[:, :], False, True, [r_ustrict, r_sel], [r_tps])
                        STT(slotfull[:, :], tps[:, 0:256], float(NE), iota_e[:, :], ALU.mult, ALU.add, [r_tps, r_iota], [r_slotfull])
                        TT("dve", selcum[:, :], selcum[:, :], sel[:, :], ALU.add, [r_selcum, r_sel], [r_selcum])
                        for k in range(8):
                            col = it * 8 + k
                            STT(junk[:, :], gs[:, :], v8s[:, k:k + 1], slotfull[:, :], ALU.is_equal, ALU.mult,
                                [r_gs, r_v8s, r_slotfull], [r_junk, r_slot8f], accum_out=slot8f[:, col:col + 1])
                        CP("dve", slot8i[:, it * 8:(it + 1) * 8], slot8f[:, it * 8:(it + 1) * 8], [r_slot8f], [r_slot8i])
                        for k in range(8):
                            col = it * 8 + k
                            S.op("pool", lambda e, col=col, it=it: e.indirect_dma_start(
                                out=slot_tok, out_offset=bass.IndirectOffsetOnAxis(ap=slot8i[:, col:col + 1], axis=0),
                                in_=tokid[:, it, :], in_offset=None, bounds_check=BC(e), oob_is_err=False),
                                [r_slot8i, r_tokid, r_slotinit, r_slotinit_b], [Res(f"slot{col}", semgrp=g_slot)], dma=True)

                    if stage != "h" and prev[0] is not None:
                        prev[0]()
                        if prev[2] is not None:
                            prev[2]()
                    part2()
                    if stage == "h":
                        DMA("sp", out[it * 128:(it + 1) * 128, :], hcur[:, :], [r_hcur], [Res(f"out{it}", semgrp=g_out)])
                        continue
                    if prev[1] is not None:
                        prev[1]()
                    prev[0], prev[1], prev[2] = route_a, route_b, None
                def shared_blk(blk=blk):
                    for j in range(2):
                        tg, r_tg = PS()
                        tu, r_tu = PS()
                        for kc in range(8):
                            MM(tg[:, 0:512], wsgu[:, kc, j * 128:(j + 1) * 128], hTb[:, kc, :], kc == 0, kc == 7, r_wsgu + [r_hTb], [r_tg])
                        for kc in range(8):
                            MM(tu[:, 0:512], wsgu[:, kc, 256 + j * 128:256 + (j + 1) * 128], hTb[:, kc, :], kc == 0, kc == 7,
                               r_wsgu + [r_hTb], [r_tu])
                        ACT(sg[:, :], tg[:, 0:512], AF.Silu, [r_tg], [r_sg])
                        TT("dve", actsh[:, j, :], sg[:, :], tu[:, 0:512], ALU.mult, [r_sg, r_tu], [r_actsh])
                    for t4 in range(4):
                        it = blk * 4 + t4
                        cols = slice(t4 * 128, (t4 + 1) * 128)
                        hcur, r_hcur = hring[it % 4], r_hring[it % 4]
                        ty0, r_ty0 = PS()
                        ty1, r_ty1 = PS()
                        for kc in range(2):
                            MM(ty0[:, 0:512], actsh[:, kc, cols], wsd_bf[:, kc, 0:512], kc == 0, kc == 1, [r_actsh, r_wsd], [r_ty0])
                            MM(ty1[:, 0:512], actsh[:, kc, cols], wsd_bf[:, kc, 512:1024], kc == 0, kc == 1, [r_actsh, r_wsd], [r_ty1])
                        zp = it % 2
                        STT(za[zp][:, 0:512], hcur[:, 0:512], ALPHA, ty0[:, 0:512], ALU.mult, ALU.add, [r_hcur, r_ty0], [r_za[zp]])
                        STT(za[zp][:, 512:1024], hcur[:, 512:1024], ALPHA, ty1[:, 0:512], ALU.mult, ALU.add, [r_hcur, r_ty1], [r_za[zp]])
                        DMA("sp", zacc[it * 128:(it + 1) * 128, :], za[zp][:, :], [r_za[zp]], [Res(f"zacc{it}", semgrp=g_zacc)])

                if stage != "h":
                    prev[2] = shared_blk
            if stage != "h":
                prev[0]()
                prev[2]()
                prev[1]()
            S.barrier()
            S.emit()

        if stage == "h":
            return nc

        with ExitStack() as st2:
            sb = mk(st2)
            st_v = slot_tok.rearrange("(s e) o -> s (e o)", e=NE)
            tbl, r_tbl = sb("tbl", [128, 2 * NE], I32), Res("tbl")
            tblB, r_tblB = sb("tblB", [128, 2 * NE], I32), Res("tblB")
            DMA("sp", tblB[:], c_tblinit, [], [r_tblB])
            DMA("sp", tblB[0:CAP - 128, :], st_v[128:CAP, :], [], [r_tblB])
            g2, r_g2 = sb("g2", [128, 1024]), Res("g2")
            DMA("sp", g2[:], ln2g, [], [r_g2])
            b2, r_b2 = sb("b2", [128, 1024]), Res("b2")
            DMA("sp", b2[:], ln2b, [], [r_b2])
            DMA("sp", tbl[:], st_v[0:128, :], [], [r_tbl])
            if stage == "route":
                f1 = DMA("sp", dbg_tbl, tbl[:], [r_tbl, r_tblB], [Res("dbg_tbl")])
                f2 = DMA("sp", dbg_slot, slot8f[:], [r_slot8f], [Res("dbg_slot")])
                f3 = DMA("sp", dbg_w8, w8[:], [r_w8], [Res("dbg_w8")])
                zt_, r_zt = sb("zt_", [128, 1024]), Res("zt_")
                fl = [f1, f2, f3]
                for it in range(NT):
                    DMA("sp", zt_[:, :], zacc[it * 128:(it + 1) * 128, :], [], [r_zt])
                    fl.append(DMA("sp", out[it * 128:(it + 1) * 128, :], zt_[:, :], [r_zt], [Res(f"out{it}", semgrp=g_out)]))
                S.wait_final("sp", fl)
                S.emit()
                return nc
            NB = 3
            wgu = [sb(f"wgu{i}", [128, 8, 512], BF16) for i in range(NB)]
            r_wg = [Res(f"wg{i}") for i in range(NB)]
            r_wu = [Res(f"wu{i}") for i in range(NB)]
            wd = [sb(f"wd{i}", [128, 2, 1024], BF16) for i in range(NB)]
            r_wd = [Res(f"wd{i}") for i in range(NB)]
            xg = [sb(f"xg{i}", [128, 1024], BF16) for i in range(2)]
            r_xg = [Res(f"xg{i}") for i in range(2)]
            for i in range(2):
                S.op("dve", lambda e, i=i: e.memset(xg[i][:], 0.0), [], [r_xg[i]])
            xgT = [sb(f"xgT{i}", [128, 8, 128], BF16) for i in range(2)]
            r_xgT = [Res(f"xgT{i}") for i in range(2)]
            sge = [sb(f"sge{i}", [128, 256]) for i in range(2)]
            r_sge = [Res(f"sge{i}") for i in range(2)]
            ae = [sb(f"ae{i}", [128, 256], BF16) for i in range(2)]
            r_ae = [Res(f"ae{i}") for i in range(2)]
            aT = [sb(f"aT{i}", [128, 2, 128], BF16) for i in range(2)]
            r_aT = [Res(f"aT{i}") for i in range(2)]
            NY = 4
            ysb = [sb(f"ysb{i}", [128, 1024]) for i in range(NY)]
            r_ysb = [Res(f"ysb{i}") for i in range(NY)]
            g_ys = [Res(f"yslots{i}") for i in range(NY)]
            r_ys_all = []
            ys_v = yslots.rearrange("(s e) d -> s e d", e=NE)

            def load_w(e_):
                wb = e_ % NB
                DMA("pool", wgu[wb][:, :, 0:256], weg[e_].rearrange("(c p) n -> p c n", p=128), [], [r_wg[wb]])
                DMA("pool", wgu[wb][:, :, 256:512], weu[e_].rearrange("(c p) n -> p c n", p=128), [], [r_wu[wb]])
                DMA("pool", wd[wb][:], wed[e_].rearrange("(c p) n -> p c n", p=128), [], [r_wd[wb]])

            def stage_a(e_, blk, j):
                wb = e_ % NB
                p2 = j % 2
                tb_src, r_tb_src = (tbl, r_tbl) if blk == 0 else (tblB, r_tblB)
                S.op("pool", lambda e: e.indirect_dma_start(
                    out=xg[p2][:, :], out_offset=None, in_=hbf,
                    in_offset=bass.IndirectOffsetOnAxis(ap=tb_src[:, 2 * e_:2 * e_ + 1], axis=0),
                    bounds_check=BCG(e), oob_is_err=False),
                    [r_tb_src], [r_xg[p2]], dma=True)
                t, r = PS()
                tb_ = t[:, :].bitcast(BF16)
                for c in range(8):
                    TR(tb_[:, c * 128:(c + 1) * 128], xg[p2][:, c * 128:(c + 1) * 128], identb[:, :], [r_xg[p2], r_identb], [r])
                CP("dve", xgT[p2][:, :, :], tb_[:, 0:1024].rearrange("p (a t) -> p a t", a=8), [r], [r_xgT[p2]])
                t2, r2 = PS()
                for kc in range(8):
                    MM(t2[:, 0:512], xgT[p2][:, kc, :], wgu[wb][:, kc, :], kc == 0, kc == 7, [r_xgT[p2], r_wg[wb], r_wu[wb]], [r2])
                ACT(sge[p2][:, :], t2[:, 0:256], AF.Silu, [r2], [r_sge[p2]])
                TT("dve", ae[p2][:, :], sge[p2][:, :], t2[:, 256:512], ALU.mult, [r_sge[p2], r2], [r_ae[p2]])

            def stage_b(e_, blk, j):
                wb = e_ % NB
                p2 = j % 2
                t, r = PS()
                tb_ = t[:, :].bitcast(BF16)
                for c in range(2):
                    TR(tb_[:, c * 128:(c + 1) * 128], ae[p2][:, c * 128:(c + 1) * 128], identb[:, :], [r_ae[p2], r_identb], [r])
                ACT(aT[p2][:, :, :], tb_[:, 0:256].rearrange("p (a t) -> p a t", a=2), AF.Copy, [r], [r_aT[p2]])
                ty0, r_ty0 = PS()
                ty1, r_ty1 = PS()
                for kc in range(2):
                    MM(ty0[:, 0:512], aT[p2][:, kc, :], wd[wb][:, kc, 0:512], kc == 0, kc == 1, [r_aT[p2], r_wd[wb]], [r_ty0])
                    MM(ty1[:, 0:512], aT[p2][:, kc, :], wd[wb][:, kc, 512:1024], kc == 0, kc == 1, [r_aT[p2], r_wd[wb]], [r_ty1])
                py = j % NY
                nr = 128 if blk == 0 else CAP - 128
                ACT(ysb[py][0:nr, 0:512], ty0[0:nr, 0:512], AF.Copy, [r_ty0], [r_ysb[py]])
                CP("dve", ysb[py][0:nr, 512:1024], ty1[0:nr, 0:512], [r_ty1], [r_ysb[py]])
                ry = Res(f"ys{e_}_{blk}", semgrp=g_ys[py])
                r_ys_all.append(ry)
                s0 = 0 if blk == 0 else 128
                DMA("sp", ys_v[s0:s0 + nr, e_, :], ysb[py][0:nr, :], [r_ysb[py]], [ry])

            work = [(e_, blk) for e_ in range(n_exp) for blk in range(2)]
            for j in range(len(work) + 1):
                if j < len(work):
                    if work[j][1] == 0:
                        load_w(work[j][0])
                    stage_a(work[j][0], work[j][1], j)
                if j >= 1:
                    stage_b(work[j - 1][0], work[j - 1][1], j - 1)

            acc = [sb(f"acc{i}", [128, 1024]) for i in range(2)]
            r_acc = [Res(f"acc{i}") for i in range(2)]
            NYK = 8
            yk = [sb(f"yk{i}", [128, 1024]) for i in range(NYK)]
            r_yk = [Res(f"yk{i}") for i in range(NYK)]
            lnt2, r_lnt2 = None, None
            ot = [sb(f"ot{i}", [128, 1024]) for i in range(2)]
            r_ot = [Res(f"ot{i}") for i in range(2)]
            st6b, r_st6b = sb("st6b", [128, 2, 6]), Res("st6b")
            mvb, r_mvb = sb("mvb", [128, 2]), Res("mvb")
            smb, r_smb = sb("smb", [128, 4]), Res("smb")
            finals = []
            gk = 0
            for it in range(NT):
                p2 = it % 2
                DMA("sp", acc[p2][:, :], zacc[it * 128:(it + 1) * 128, :], [], [r_acc[p2]])
                for k in range(8):
                    col = it * 8 + k
                    yb = gk % NYK
                    gk += 1
                    S.op("pool", lambda e, yb=yb, col=col: e.indirect_dma_start(
                        out=yk[yb][:, :], out_offset=None, in_=yslots,
                        in_offset=bass.IndirectOffsetOnAxis(ap=slot8i[:, col:col + 1], axis=0),
                        bounds_check=BC(e), oob_is_err=False),
                        [r_slot8i] + r_ys_all, [r_yk[yb]], dma=True)
                    STT(acc[p2][:, :], yk[yb][:, :], w8[:, col:col + 1], acc[p2][:, :], ALU.mult, ALU.add,
                        [r_yk[yb], r_w8, r_acc[p2]], [r_acc[p2]])
                layer_norm(acc[p2], r_acc[p2], g2, r_g2, b2, r_b2, ot[p2], r_ot[p2], lnt2, r_lnt2, st6b, r_st6b, mvb, r_mvb, smb, r_smb)
                finals.append(DMA("sp", out[it * 128:(it + 1) * 128, :], ot[p2][:, :], [r_ot[p2]], [Res(f"out{it}", semgrp=g_out2[p2])]))
            S.wait_final("sp", finals)
            S.emit()
    return nc


def _consts(half):
    c = {}
    tp = np.arange(128)[:, None]
    t = np.arange(128)[None, :]
    mcur = np.zeros((128, 4, 128), np.float32)
    mprev = np.zeros((128, 4, 128), np.float32)
    mfirst = np.zeros((128, 4, 128), np.float32)
    for g, w in enumerate((2, 4, 8, 16)):
        band = ((tp <= t) & (tp > t - w)).astype(np.float32)
        mcur[:, g, :] = band / w - (tp == t)
        mprev[:, g, :] = (((tp - 128) <= t) & ((tp - 128) > t - w)).astype(np.float32) / w
        cnt = np.minimum(t + 1, w).astype(np.float32)
        mfirst[:, g, :] = band / cnt - (tp == t)
    c["c_mcur"] = mcur
    c["c_mprev"] = mprev
    c["c_mfirst"] = mfirst if half == 0 else mcur
    c["c_uinc"] = (tp <= t).astype(np.float32) * (-1.0 / 16.0)
    c["c_rgt"] = (tp > t).astype(np.float32) * (-1.0 / 16.0)
    c["c_mask4"] = np.repeat((tp <= t).astype(np.float32)[:, None, :], 4, axis=1)
    c["c_ident"] = np.eye(128, dtype=np.float32)
    c["c_ustrict"] = (tp < t).astype(np.float32)
    c["c_ones"] = np.ones((128, 128), np.float32)
    c["c_iota"] = np.broadcast_to(np.arange(256, dtype=np.float32)[None, :], (128, 256)).copy()
    tk = (np.arange(NT, dtype=np.int32)[None, :] * 128 + np.arange(128, dtype=np.int32)[:, None]).astype(np.int32)
    c["c_tokid"] = np.repeat(tk[:, :, None], 2, axis=2)
    c["c_tblinit"] = np.full((128, 512), PAD_IDX, np.int32)
    return {k: np.ascontiguousarray(v) for k, v in c.items()}


def _relayout_gu(wg, wu):
    E = wg.shape[0]
    o = np.empty((E, 128, 8, 512), np.float32)
    o[:, :, :, 0:256] = wg.reshape(E, 8, 128, 256).transpose(0, 2, 1, 3)
    o[:, :, :, 256:512] = wu.reshape(E, 8, 128, 256).transpose(0, 2, 1, 3)
    return o.reshape(E, 128, 8 * 512)


def _relayout_d(wd):
    E = wd.shape[0]
    return np.ascontiguousarray(wd.reshape(E, 2, 128, 1024).transpose(0, 2, 1, 3)).reshape(E, 128, 2 * 1024)


def make_in_maps(x, w_in, gla_gate_w2, gla_gate_b, gla_norm_w, pool_w_group, pool_scale, w_out, ln1_g, ln1_b,
                 router_w, router_bias, w_exp_gate, w_exp_up, w_exp_down, w_sh_gate, w_sh_up, w_sh_down, ln2_g, ln2_b):
    f = lambda a: np.ascontiguousarray(np.asarray(a, dtype=np.float32))
    x = f(x)
    w_in0 = f(w_in)[0]
    wgl = np.zeros((1024, 128), np.float32)
    wgl[:, 0:16] = w_in0[:, 2048:2064]
    w2p = np.zeros((128, 256), np.float32)
    w2p[0:16] = f(gla_gate_w2)[0]
    w2p[32] = f(gla_gate_b)[0]
    bc = lambda v, n: np.ascontiguousarray(np.broadcast_to(f(v)[0][None, :], (128, n)))
    shared = {
        "w_in": w_in0, "wgl": wgl, "w2p": w2p,
        "normw": f(gla_norm_w)[0].reshape(128, 1).copy(),
        "poolw": f(pool_w_group)[0],
        "pscale": np.ascontiguousarray(f(pool_scale)[0].reshape(4, 128).T),
        "w_out": f(w_out)[0],
        "ln1g": bc(ln1_g, 1024), "ln1b": bc(ln1_b, 1024), "ln2g": bc(ln2_g, 1024), "ln2b": bc(ln2_b, 1024),
        "router_w": f(router_w)[0], "rbias": bc(router_bias, 256),
        "weg": f(w_exp_gate)[0], "weu": f(w_exp_up)[0], "wed": f(w_exp_down)[0],
        "wsg": f(w_sh_gate)[0], "wsu": f(w_sh_up)[0], "wsd": f(w_sh_down)[0],
    }
    cst = [_consts(0), _consts(1)]
    in_maps = []
    for c in range(NCORES):
        b, half = c // 2, c % 2
        xT = np.zeros((1024, 2 * TL), np.float32)
        if half == 1:
            xT[:, 0:TL] = x[b, 0:TL].T
        xT[:, TL:] = x[b, half * TL:(half + 1) * TL].T
        m = dict(shared)
        m.update(cst[half])
        m["xT"] = xT
        m["xtok"] = np.ascontiguousarray(x[b, half * TL:(half + 1) * TL])
        in_maps.append(m)
    return in_maps


_NC_CACHE = {}


def kernel(**inputs):
    in_maps = make_in_maps(**inputs)
    if "full" not in _NC_CACHE:
        _NC_CACHE["full"] = build("full")
    nc = _NC_CACHE["full"]
    res = run_bass_kernel_spmd(nc, in_maps, core_ids=list(range(NCORES)))
    outs = [np.asarray(r["out"], dtype=np.float32) for r in res.results]
    full = np.zeros((4, 4096, 1024), np.float32)
    for c in range(NCORES):
        b, half = c // 2, c % 2
        full[b, half * TL:(half + 1) * TL] = outs[c]
    return full
```

```python
import numpy as np
from contextlib import ExitStack
import concourse.bass as bass
import concourse.mybir as mybir
from concourse.bass_utils import run_bass_kernel_spmd

F32 = mybir.dt.float32
BF16 = mybir.dt.bfloat16
I32 = mybir.dt.int32
AF = mybir.ActivationFunctionType
ALU = mybir.AluOpType
AX = mybir.AxisListType

NCORES = 8
TL = 2048
NT = TL // 128
NE = 256
CAP = 192
ALPHA = 2.0 ** 0.25
LN_EPS = 1e-5
RMS_EPS = 1e-5
BIG = 1.0e9
PAD_IDX = 1 << 30


class Res:
    __slots__ = ("name", "last_write", "reads", "semgrp", "sem", "cnt", "excl")

    def __init__(self, name, semgrp=None, excl=False):
        self.name = name
        self.excl = excl
        self.last_write = None
        self.reads = []
        self.semgrp = semgrp if semgrp is not None else self
        self.sem = None
        self.cnt = 0


class Sched:
    ENGS = ("pe", "act", "dve", "pool", "sp")

    def __init__(self, nc, stack):
        self.nc = nc
        self.stack = stack
        self.q = {e: [] for e in self.ENGS}
        self.cnt = {e: 0 for e in self.ENGS}
        self.sem = {e: stack.enter_context(nc.semaphore("prog_" + e)) for e in self.ENGS}
        self.waited = {e: {} for e in self.ENGS}
        self.groups = []

    def _dma_grp(self, res):
        g = res.semgrp
        if g.sem is None:
            g.sem = self.stack.enter_context(self.nc.semaphore("dma_" + g.name))
            self.groups.append(g)
        return g

    def op(self, eng, fn, reads=(), writes=(), dma=False):
        xr = [r for r in reads if r.excl and r not in writes]
        if xr:
            reads = [r for r in reads if not r.excl]
            writes = list(writes) + xr
        deps = set()
        for r in reads:
            if r.last_write is not None:
                deps.add(r.last_write)
        for w in writes:
            if w.last_write is not None:
                deps.add(w.last_write)
            deps.update(w.reads)
        if dma:
            g = self._dma_grp(writes[0])
            g.cnt += 16
            token = (g.sem, g.cnt)
        else:
            self.cnt[eng] += 1
            token = (self.sem[eng], self.cnt[eng])
        if eng == "pe":
            deps = {d for d in deps if d[0] is not self.sem["pe"]}
        self.q[eng].append((fn, deps, token, dma))
        for r in reads:
            r.reads.append(token)
        for w in writes:
            w.last_write = token
            w.reads = []
        return token

    def barrier(self):
        toks = [(self.sem[e], self.cnt[e]) for e in self.ENGS if self.cnt[e] > 0]
        toks += [(g.sem, g.cnt) for g in self.groups]
        for e in self.ENGS:
            self.q[e].append((None, set(toks), None, False))

    def wait_final(self, eng, tokens):
        self.q[eng].append((None, set(tokens), None, False))

    def emit(self):
        nc = self.nc
        with nc.Block() as block:
            def run(engname, eng):
                waited = self.waited[engname]
                for (fn, deps, token, dma) in self.q[engname]:
                    best = {}
                    for (s, v) in deps:
                        if v > best.get(s.num, (None, 0))[1]:
                            best[s.num] = (s, v)
                    for k, (s, v) in best.items():
                        if waited.get(k, 0) >= v:
                            continue
                        eng.wait_ge(s, v)
                        waited[k] = v
                    if fn is None:
                        continue
                    ins = fn(eng)
                    ins.then_inc(token[0], 16 if dma else 1)
                self.q[engname] = []

            @block.tensor
            def _(e):
                run("pe", e)

            @block.scalar
            def _(e):
                run("act", e)

            @block.vector
            def _(e):
                run("dve", e)

            @block.gpsimd
            def _(e):
                run("pool", e)

            @block.sync
            def _(e):
                run("sp", e)


def build(stage="full", n_exp=NE):
    nc = bass.Bass("TRN2", target_bir_lowering=False)

    def din(name, shape, dt=F32):
        return nc.dram_tensor(name, list(shape), dt, kind="ExternalInput").ap()

    xT = din("xT", [1024, 2 * TL])
    xtok = din("xtok", [TL, 1024])
    w_in = din("w_in", [1024, 2064])
    wgl = din("wgl", [1024, 128])
    w2p = din("w2p", [128, 256])
    normw = din("normw", [128, 1])
    poolw = din("poolw", [4, 128, 128])
    pscale = din("pscale", [128, 4])
    w_out = din("w_out", [1024, 1024])
    ln1g = din("ln1g", [128, 1024])
    ln1b = din("ln1b", [128, 1024])
    ln2g = din("ln2g", [128, 1024])
    ln2b = din("ln2b", [128, 1024])
    router_w = din("router_w", [1024, 256])
    rbias = din("rbias", [128, 256])
    if stage == "full":
        weg = din("weg", [NE, 1024, 256])
        weu = din("weu", [NE, 1024, 256])
        wed = din("wed", [NE, 256, 1024])
    if stage == "route":
        dbg_slot = nc.dram_tensor("dbg_slot", [128, NT * 8], F32, kind="ExternalOutput").ap()
        dbg_w8 = nc.dram_tensor("dbg_w8", [128, NT * 8], F32, kind="ExternalOutput").ap()
        dbg_tbl = nc.dram_tensor("dbg_tbl", [128, 2 * NE], I32, kind="ExternalOutput").ap()
    wsg = din("wsg", [1024, 256])
    wsu = din("wsu", [1024, 256])
    wsd = din("wsd", [256, 1024])
    c_mcur = din("c_mcur", [128, 4, 128])
    c_mprev = din("c_mprev", [128, 4, 128])
    c_mfirst = din("c_mfirst", [128, 4, 128])
    c_uinc = din("c_uinc", [128, 128])
    c_rgt = din("c_rgt", [128, 128])
    c_mask4 = din("c_mask4", [128, 4, 128])
    c_ident = din("c_ident", [128, 128])
    c_ustrict = din("c_ustrict", [128, 128])
    c_ones = din("c_ones", [128, 128])
    c_iota = din("c_iota", [128, 256])
    c_tokid = din("c_tokid", [128, NT, 2], I32)
    c_tblinit = din("c_tblinit", [128, 512], I32)
    out = nc.dram_tensor("out", [TL, 1024], F32, kind="ExternalOutput").ap()

    hbf = nc.dram_tensor("hbf", [TL + 1, 1024], BF16).ap()
    zacc = nc.dram_tensor("zacc", [TL, 1024], F32).ap()
    slot_tok = nc.dram_tensor("slot_tok", [CAP * NE, 2], I32).ap()
    yslots = nc.dram_tensor("yslots", [CAP * NE, 1024], F32).ap()

    with ExitStack() as st0:
        S = Sched(nc, st0)

        def mk(st):
            def sb(name, shape, dt=F32):
                return st.enter_context(nc.sbuf_tensor(name, list(shape), dt))
            return sb

        sb0 = mk(st0)

        def MM(o, lhsT, rhs, start, stop, r, w):
            S.op("pe", lambda e: e.matmul(o, lhsT, rhs, start=start, stop=stop), r, w)

        def TR(o, in_, ident, r, w):
            S.op("pe", lambda e: e.transpose(o, in_, ident), r, w)

        def ACT(o, in_, func, r, w, **kw):
            return S.op("act", lambda e: e.activation(out=o, in_=in_, func=func, **kw), r, w)

        def TT(eng, o, a, b, op, r, w):
            return S.op(eng, lambda e: e.tensor_tensor(out=o, in0=a, in1=b, op=op), r, w)

        def TS(eng, o, a, s1, s2, op0, op1, r, w, **kw):
            if op1 is None:
                return S.op(eng, lambda e: e.tensor_scalar(out=o, in0=a, scalar1=s1, scalar2=None, op0=op0, **kw), r, w)
            return S.op(eng, lambda e: e.tensor_scalar(out=o, in0=a, scalar1=s1, scalar2=s2, op0=op0, op1=op1, **kw), r, w)

        def STT(o, a, s, b, op0, op1, r, w, **kw):
            return S.op("dve", lambda e: e.scalar_tensor_tensor(out=o, in0=a, scalar=s, in1=b, op0=op0, op1=op1, **kw), r, w)

        def CP(eng, o, a, r, w):
            return S.op(eng, lambda e: e.tensor_copy(out=o, in_=a), r, w)

        def DMA(q, o, in_, r, w, **kw):
            return S.op(q, lambda e: e.dma_start(out=o, in_=in_, **kw), r, w, dma=True)

        bc_reg = {}

        def BCG(e):
            if "g" not in bc_reg:
                bc_reg["g"] = e.alloc_register("bcg")
                e.reg_mov(bc_reg["g"], TL - 1)
            return bc_reg["g"]

        def BC(e):
            if "r" not in bc_reg:
                bc_reg["r"] = e.alloc_register("bc")
                e.reg_mov(bc_reg["r"], CAP * NE - 1)
            return bc_reg["r"]

        banks = []
        for i in range(8):
            t = st0.enter_context(nc.psum_tensor(f"bank{i}", [128, 512], F32))
            banks.append((t, Res(f"bank{i}", excl=True)))
        bk = [0]

        def PS():
            b = banks[bk[0] % 8]
            bk[0] += 1
            return b

        def const_load(name, src, shape, dt=F32, q="sp"):
            t = sb0(name, shape, dt)
            r = Res(name)
            DMA(q, t[:], src, [], [r])
            return t, r

        ident32, r_ident32 = const_load("ident32", c_ident, [128, 128])
        identb, r_identb = const_load("identb", c_ident, [128, 128], BF16, "pool")
        tokid, r_tokid = const_load("tokid", c_tokid, [128, NT, 2], I32)
        slot8f = sb0("slot8f", [128, NT * 8]); r_slot8f = Res("slot8f")
        slot8i = sb0("slot8i", [128, NT * 8], I32); r_slot8i = Res("slot8i")
        w8 = sb0("w8", [128, NT * 8]); r_w8 = Res("w8")
        g_out = Res("outgrp")
        g_out2 = [Res("outgrp0"), Res("outgrp1")]
        g_hbf = Res("hbf")
        g_zacc = Res("zacc")
        g_slot = Res("slot_tok")
        r_slotinit = Res("slot_init")

        ln_eng = ["dve"]

        def layer_norm(z, r_z, gam, r_gam, bet, r_bet, o, r_o, tmp, r_tmp, st6, r_st6, mv, r_mv, sm, r_sm):
            S.op("dve", lambda e: e.bn_stats(out=st6[:, 0, :], in_=z[:, 0:512]), [r_z], [r_st6])
            S.op("dve", lambda e: e.bn_stats(out=st6[:, 1, :], in_=z[:, 512:1024]), [r_z], [r_st6])
            S.op("dve", lambda e: e.bn_aggr(out=mv[:, :], in_=st6[:, :, :]), [r_st6], [r_mv])
            ACT(sm[:, 0:1], mv[:, 1:2], AF.Ln, [r_mv], [r_sm], bias=LN_EPS)
            ACT(sm[:, 1:2], sm[:, 0:1], AF.Exp, [r_sm], [r_sm], scale=-0.5)
            TS("dve", sm[:, 2:3], mv[:, 0:1], sm[:, 1:2], -1.0, ALU.mult, ALU.mult, [r_mv, r_sm], [r_sm])
            ACT(o[:, :], z[:, :], AF.Identity, [r_z, r_sm], [r_o], scale=sm[:, 1:2], bias=sm[:, 2:3])
            TT(ln_eng[0], o[:, :], o[:, :], gam[:, :], ALU.mult, [r_o, r_gam], [r_o])
            TT(ln_eng[0], o[:, :], o[:, :], bet[:, :], ALU.add, [r_o, r_bet], [r_o])

        with ExitStack() as st1:
            sb = mk(st1)
            w_in_bf = sb("w_in_bf", [128, 8, 2064], BF16)
            r_win = [Res("win_a"), Res("win_b")]
            w_in_v = w_in.rearrange("(c p) n -> p c n", p=128)
            DMA("pool", w_in_bf[:, :, 0:1024], w_in_v[:, :, 0:1024], [], [r_win[0]])
            DMA("pool", w_in_bf[:, :, 1024:2064], w_in_v[:, :, 1024:2064], [], [r_win[1]])
            wgl_bf = sb("wgl_bf", [128, 8, 128], BF16); r_wgl = Res("wgl")
            DMA("pool", wgl_bf[:], wgl.rearrange("(c p) n -> p c n", p=128), [], [r_wgl])
            mcur, r_mcur = sb("mcur", [128, 4, 128], BF16), Res("mcur")
            DMA("pool", mcur[:], c_mcur, [], [r_mcur])
            mprev, r_mprev = sb("mprev", [128, 4, 128], BF16), Res("mprev")
            DMA("pool", mprev[:], c_mprev, [], [r_mprev])
            mfirst, r_mfirst = sb("mfirst", [128, 4, 128], BF16), Res("mfirst")
            DMA("pool", mfirst[:], c_mfirst, [], [r_mfirst])
            poolw_bf, r_poolw = sb("poolw_bf", [128, 4, 128], BF16), Res("poolw")
            DMA("pool", poolw_bf[:], poolw.rearrange("g c d -> c g d"), [], [r_poolw])
            w_out_bf, r_wout = sb("w_out_bf", [128, 8, 1024], BF16), Res("wout")
            DMA("pool", w_out_bf[:], w_out.rearrange("(c p) n -> p c n", p=128), [], [r_wout])
            wsgu, r_wsgu = sb("wsgu", [128, 8, 512], BF16), [Res("wsg"), Res("wsu")]
            DMA("pool", wsgu[:, :, 0:256], wsg.rearrange("(c p) n -> p c n", p=128), [], [r_wsgu[0]])
            DMA("pool", wsgu[:, :, 256:512], wsu.rearrange("(c p) n -> p c n", p=128), [], [r_wsgu[1]])
            wsd_bf, r_wsd = sb("wsd_bf", [128, 2, 1024], BF16), Res("wsd")
            DMA("pool", wsd_bf[:], wsd.rearrange("(c p) n -> p c n", p=128), [], [r_wsd])
            rw_sb, r_rw = sb("rw_sb", [128, 8, 256]), Res("rw")
            DMA("sp", rw_sb[:], router_w.rearrange("(c p) n -> p c n", p=128), [], [r_rw])
            uinc, r_uinc = sb("uinc", [128, 128]), Res("uinc")
            DMA("sp", uinc[:], c_uinc, [], [r_uinc])
            rgt, r_rgt = sb("rgt", [128, 128]), Res("rgt")
            DMA("sp", rgt[:], c_rgt, [], [r_rgt])
            mask4, r_mask4 = sb("mask4", [128, 4, 128], BF16), Res("mask4")
            DMA("pool", mask4[:], c_mask4, [], [r_mask4])
            ustrict, r_ustrict = sb("ustrict", [128, 128]), Res("ustrict")
            DMA("sp", ustrict[:], c_ustrict, [], [r_ustrict])
            ones32, r_ones = sb("ones32", [128, 128]), Res("ones")
            DMA("sp", ones32[:], c_ones, [], [r_ones])
            iota_e, r_iota = sb("iota_e", [128, 256]), Res("iota")
            DMA("sp", iota_e[:], c_iota, [], [r_iota])
            w2p_sb, r_w2p = sb("w2p_sb", [128, 256]), Res("w2p")
            DMA("sp", w2p_sb[:], w2p, [], [r_w2p])
            normw_sb, r_normw = sb("normw_sb", [128, 1]), Res("normw")
            DMA("sp", normw_sb[:], normw, [], [r_normw])
            pscale_sb, r_pscale = sb("pscale_sb", [128, 4]), Res("pscale")
            DMA("sp", pscale_sb[:], pscale, [], [r_pscale])
            rbias_sb, r_rbias = sb("rbias_sb", [128, 256]), Res("rbias")
            DMA("sp", rbias_sb[:], rbias, [], [r_rbias])
            g1, r_g1 = sb("g1", [128, 1024]), Res("g1")
            DMA("sp", g1[:], ln1g, [], [r_g1])
            b1, r_b1 = sb("b1", [128, 1024]), Res("b1")
            DMA("sp", b1[:], ln1b, [], [r_b1])
            tblinit, r_tblinit = sb("tblinit", [128, 512], I32), Res("tblinit")
            DMA("sp", tblinit[:], c_tblinit, [], [r_tblinit])
            st_v = slot_tok.rearrange("(s e) o -> s (e o)", e=NE)
            DMA("sp", st_v[0:128, :], tblinit[:], [r_tblinit], [r_slotinit])
            r_slotinit_b = Res("slot_init_b")
            DMA("sp", st_v[128:CAP, :], tblinit[0:CAP - 128, :], [r_tblinit], [r_slotinit_b])

            xTb = [sb(f"xTb{i}", [128, 8, 512], BF16) for i in range(2)]
            r_xTb = [Res(f"xTb{i}") for i in range(2)]
            glT, r_glT = sb("glT", [128, 512]), Res("glT")
            S.op("dve", lambda e: e.memset(glT[:], 0.0), [], [r_glT])
            S.op("dve", lambda e: e.memset(glT[32:33, :], 1.0), [], [r_glT])
            DMA("sp", hbf[TL:TL + 1, :], glT[64:65, :].bitcast(BF16), [r_glT], [Res("hbf_zero", semgrp=g_hbf)])
            vtok = [sb(f"vtok{i}", [128, 512], BF16) for i in range(2)]
            r_vtok = [Res(f"vtok{i}") for i in range(2)]
            ptok = [sb(f"ptok{i}", [128, 512], BF16) for i in range(2)]
            r_ptok = [Res(f"ptok{i}") for i in range(2)]
            S.op("dve", lambda e: e.memset(ptok[0][:], 0.0), [], [r_ptok[0]])
            S.op("dve", lambda e: e.memset(ptok[1][:], 0.0), [], [r_ptok[1]])
            ez, r_ez = sb("ez", [128, 256]), Res("ez")
            sp_, r_sp = sb("sp_", [128, 256]), Res("sp_")
            eb, r_eb = sb("eb", [128, 256]), Res("eb")
            kdec, r_kdec = sb("kdec", [128, 256], BF16), Res("kdec")
            dec, r_dec = sb("dec", [128, 4]), Res("dec")
            S32, r_S32 = sb("S32", [128, 2, 256]), Res("S32")
            S.op("dve", lambda e: e.memset(S32[:], 0.0), [], [r_S32])
            Sbf, r_Sbf = sb("Sbf", [128, 2, 256], BF16), Res("Sbf")
            qT32, r_qT32 = sb("qT32", [128, 2, 512]), Res("qT32")
            kT32, r_kT32 = sb("kT32", [128, 2, 512]), Res("kT32")
            srT, r_srT = sb("srT", [128, 4, 512]), Res("srT")
            ebT, r_ebT = sb("ebT", [128, 256]), Res("ebT")
            enbT, r_enbT = sb("enbT", [128, 256]), Res("enbT")
            ktT, r_ktT = sb("ktT", [128, 2, 128], BF16), Res("ktT")
            qTz = [sb(f"qTz{i}", [128, 4, 128], BF16) for i in range(2)]
            r_qTz = [Res(f"qTz{i}") for i in range(2)]
            S.op("dve", lambda e: e.memset(qTz[0][:], 0.0), [], [r_qTz[0]])
            S.op("dve", lambda e: e.memset(qTz[1][:], 0.0), [], [r_qTz[1]])
            sTm, r_sTm = sb("sTm", [128, 4, 128], BF16), Res("sTm")
            osq, r_osq = sb("osq", [128, 512]), Res("osq")
            lnv, r_lnv = osq, r_osq
            rstd, r_rstd = osq, r_osq
            on_, r_on = sb("on_", [128, 512]), Res("on_")
            mixT, r_mixT = sb("mixT", [128, 4, 128], BF16), Res("mixT")
            catT, r_catT = sb("catT", [128, 8, 128], BF16), Res("catT")
            xt0 = sb("xt0", [128, 1024])
            xt = [xt0, xt0]
            r_xt0 = Res("xt0")
            r_xt = [r_xt0, r_xt0]
            z1, r_z1 = sb("z1", [128, 1024]), Res("z1")
            lnt, r_lnt = None, None
            st6, r_st6 = sb("st6", [128, 2, 6]), Res("st6")
            mv, r_mv = sb("mv", [128, 2]), Res("mv")
            sm, r_sm = sb("sm", [128, 4]), Res("sm")
            hring = [sb(f"h{i}", [128, 1024]) for i in range(4)]
            r_hring = [Res(f"h{i}") for i in range(4)]
            hb, r_hb = sb("hb", [128, 1024], BF16), Res("hb")
            hT32, r_hT32 = sb("hT32", [128, 8, 128]), Res("hT32")
            hTb, r_hTb = sb("hTb", [128, 8, 512], BF16), Res("hTb")
            scores2 = [sb(f"scores{i}", [128, 256]) for i in range(2)]
            r_scores2 = [Res(f"scores{i}") for i in range(2)]
            pending = [None]
            prev = [None, None, None]
            v8s, r_v8s = sb("v8s", [128, 8]), Res("v8s")
            biased, r_biased = sb("biased", [128, 256]), Res("biased")
            tmpA, r_tmpA = sb("tmpA", [128, 256]), Res("tmpA")
            tmpB, r_tmpB = sb("tmpB", [128, 256]), Res("tmpB")
            g8, r_g8 = sb("g8", [128, 32]), Res("g8")
            v8, r_v8 = sb("v8", [128, 8]), Res("v8")
            mbv, r_mbv = sb("mbv", [128, 256]), Res("mbv")
            sel, r_sel = sb("sel", [128, 256]), Res("sel")
            selcum, r_selcum = sb("selcum", [128, 256]), Res("selcum")
            S.op("dve", lambda e: e.memset(selcum[:], 0.0), [], [r_selcum])
            gs, r_gs = tmpB, r_tmpB
            rs, r_rs = sb("rs", [128, 2]), Res("rs")
            slotfull, r_slotfull = sb("slotfull", [128, 256]), Res("slotfull")
            junk, r_junk = tmpA, r_tmpA
            sg, r_sg = sb("sg", [128, 512]), Res("sg")
            actsh, r_actsh = sb("actsh", [128, 2, 512], BF16), Res("actsh")
            za0 = sb("za0", [128, 1024])
            za = [za0, za0]
            r_za0 = Res("za0")
            r_za = [r_za0, r_za0]

            xT_v = xT.rearrange("(c p) t -> p c t", p=128)

            def load_x_block(gblk):
                i = gblk % 2
                DMA("pool", xTb[i][:], xT_v[:, :, gblk * 512:(gblk + 1) * 512], [], [r_xTb[i]])
                return xTb[i], r_xTb[i]

            def proj_glow(xb, r_xb):
                t, r = PS()
                for kc in range(8):
                    MM(t[:, 0:512], wgl_bf[:, kc, :], xb[:, kc, :], kc == 0, kc == 7, [r_wgl, r_xb], [r])
                ACT(glT[0:32, :], t[0:32, 0:512], AF.Copy, [r], [r_glT])

            def tok_proj(xb, r_xb, cols, c0, c1, n):
                t, r = PS()
                for kc in range(8):
                    MM(t[:, 0:n], xb[:, kc, cols], w_in_bf[:, kc, c0:c1], kc == 0, kc == 7, [r_xb] + r_win, [r])
                return t, r

            def gate_and_kdec(cols, tk, r_tk):
                t, r = PS()
                MM(t[:, 0:256], glT[:, cols], w2p_sb[:, :], True, True, [r_glT, r_w2p], [r])
                ACT(ez[:, :], t[:, 0:256], AF.Exp, [r], [r_ez], scale=-1.0)
                ACT(sp_[:, :], ez[:, :], AF.Ln, [r_ez], [r_sp], bias=1.0)
                t2, r2 = PS()
                MM(t2[:, 0:256], rgt[:, :], sp_[:, :], True, True, [r_rgt, r_sp], [r2])
                ACT(eb[:, :], t2[:, 0:256], AF.Exp, [r2], [r_eb])
                TT("dve", kdec[:, :], tk[:, 0:256], eb[:, :], ALU.mult, [r_tk, r_eb], [r_kdec])

            def state_update(par, dec_ap_fn, r_decsrc):
                t, r = PS()
                for half in range(2):
                    MM(t[:, half * 256:(half + 1) * 256], kdec[:, half * 128:(half + 1) * 128],
                       vtok[par][:, half * 256:(half + 1) * 256], True, True, [r_kdec, r_vtok[par]], [r])
                for half in range(2):
                    STT(S32[:, half, :], S32[:, half, :], dec_ap_fn(half), t[:, half * 256:(half + 1) * 256],
                        ALU.mult, ALU.add, [r_S32, r_decsrc, r], [r_S32])

            gtile = 0
            for blk in range(4):
                xb, r_xb = load_x_block(blk)
                proj_glow(xb, r_xb)
                for t4 in range(4):
                    it = blk * 4 + t4
                    par = gtile % 2
                    gtile += 1
                    cols = slice(t4 * 128, (t4 + 1) * 128)
                    tv, r_tv = tok_proj(xb, r_xb, cols, 1024, 1536, 512)
                    tk, r_tk = tok_proj(xb, r_xb, cols, 768, 1024, 256)
                    ACT(vtok[par][:, :], tv[:, 0:512], AF.Copy, [r_tv], [r_vtok[par]])
                    gate_and_kdec(cols, tk, r_tk)
                    tb, r_tb = PS()
                    for half in range(2):
                        MM(tb[:, 2 * half:2 * half + 2], sp_[:, half * 128:(half + 1) * 128], uinc[:, 126:128],
                           True, True, [r_sp, r_uinc], [r_tb])
                    ACT(dec[:, :], tb[:, 0:4], AF.Exp, [r_tb], [r_dec])
                    state_update(par, lambda half: dec[:, 2 * half + 1:2 * half + 2], r_dec)
                    if it == 15:
                        tp, r_tp = tok_proj(xb, r_xb, cols, 0, 512, 512)
                        ACT(ptok[par][:, :], tp[:, 0:512], AF.Copy, [r_tp], [r_ptok[par]])
            ACT(Sbf[:, :, :], S32[:, :, :], AF.Copy, [r_S32], [r_Sbf])

            nxt = load_x_block(4)
            for blk in range(4):
                xb, r_xb = nxt
                if blk < 3:
                    nxt = load_x_block(4 + blk + 1)
                proj_glow(xb, r_xb)
                for m in range(2):
                    t, r = PS()
                    for kc in range(8):
                        MM(t[:, 0:512], w_in_bf[:, kc, 512 + m * 128:512 + (m + 1) * 128], xb[:, kc, :], kc == 0, kc == 7,
                           r_win + [r_xb], [r])
                    ACT(qT32[:, m, :], t[:, 0:512], AF.Copy, [r], [r_qT32])
                for m in range(2):
                    t, r = PS()
                    for kc in range(8):
                        MM(t[:, 0:512], w_in_bf[:, kc, 768 + m * 128:768 + (m + 1) * 128], xb[:, kc, :], kc == 0, kc == 7,
                           r_win + [r_xb], [r])
                    CP("dve", kT32[:, m, :], t[:, 0:512], [r], [r_kT32])
                for m in range(4):
                    t, r = PS()
                    for kc in range(8):
                        MM(t[:, 0:512], w_in_bf[:, kc, 1536 + m * 128:1536 + (m + 1) * 128], xb[:, kc, :], kc == 0, kc == 7,
                           r_win + [r_xb], [r])
                    ACT(srT[:, m, :], t[:, 0:512], AF.Silu, [r], [r_srT])

                for t4 in range(4):
                    it = blk * 4 + t4
                    par = gtile % 2
                    gtile += 1
                    cols = slice(t4 * 128, (t4 + 1) * 128)
                    hcur, r_hcur = hring[it % 4], r_hring[it % 4]
                    DMA("sp", xt[par][:, :], xtok[it * 128:(it + 1) * 128, :], [], [r_xt[par]])
                    tp, r_tp = tok_proj(xb, r_xb, cols, 0, 512, 512)
                    tv, r_tv = tok_proj(xb, r_xb, cols, 1024, 1536, 512)
                    tk, r_tk = tok_proj(xb, r_xb, cols, 768, 1024, 256)
                    ACT(ptok[par][:, :], tp[:, 0:512], AF.Copy, [r_tp], [r_ptok[par]])
                    ACT(vtok[par][:, :], tv[:, 0:512], AF.Copy, [r_tv], [r_vtok[par]])
                    gate_and_kdec(cols, tk, r_tk)
                    tb, r_tb = PS()
                    for half in range(2):
                        MM(tb[:, half * 128:(half + 1) * 128], sp_[:, half * 128:(half + 1) * 128], uinc[:, :],
                           True, True, [r_sp, r_uinc], [r_tb])
                    ACT(ebT[:, :], tb[:, 0:256], AF.Exp, [r_tb], [r_ebT])
                    ACT(enbT[:, :], tb[:, 0:256], AF.Exp, [r_tb], [r_enbT], scale=-1.0)
                    TT("dve", ktT[:, :, :], kT32[:, :, cols], enbT[:, :].rearrange("p (a t) -> p a t", a=2), ALU.mult,
                       [r_kT32, r_enbT], [r_ktT])
                    for h in range(4):
                        r0 = (h % 2) * 64
                        half = h // 2
                        STT(qTz[par][r0:r0 + 64, h, :], qT32[r0:r0 + 64, half, cols], 0.125,
                            ebT[r0:r0 + 64, half * 128:(half + 1) * 128], ALU.mult, ALU.mult,
                            [r_qT32, r_ebT], [r_qTz[par]])
                    ts_, r_ts = PS()
                    for h in range(4):
                        MM(ts_[:, h * 128:(h + 1) * 128], ktT[:, h // 2, :], qTz[par][:, h, :], True, True,
                           [r_ktT, r_qTz[par]], [r_ts])
                    TT("dve", sTm[:, :, :], ts_[:, 0:512].rearrange("p (a t) -> p a t", a=4), mask4[:, :, :], ALU.mult,
                       [r_ts, r_mask4], [r_sTm])
                    to, r_to = PS()
                    for h in range(4):
                        MM(to[:, h * 128:(h + 1) * 128], vtok[par][:, h * 128:(h + 1) * 128], sTm[:, h, :], True, False,
                           [r_vtok[par], r_sTm], [r_to])
                        MM(to[:, h * 128:(h + 1) * 128], Sbf[:, h // 2, (h % 2) * 128:(h % 2 + 1) * 128], qTz[par][:, h, :],
                           False, True, [r_Sbf, r_qTz[par]], [r_to])
                    ACT(osq[:, :], to[:, 0:512], AF.Square, [r_to], [r_osq])
                    tss, r_tss = PS()
                    MM(tss[:, 0:512], ones32[:, :], osq[:, :], True, True, [r_ones, r_osq], [r_tss])
                    ACT(lnv[:, :], tss[:, 0:512], AF.Ln, [r_tss], [r_lnv], scale=1.0 / 128.0, bias=RMS_EPS)
                    ACT(rstd[:, :], lnv[:, :], AF.Exp, [r_lnv], [r_rstd], scale=-0.5)
                    TT("dve", on_[:, :], to[:, 0:512], rstd[:, :], ALU.mult, [r_to, r_rstd], [r_on])
                    STT(catT[:, 4:8, :], on_[:, :].rearrange("p (a t) -> p a t", a=4), normw_sb[:, 0:1], srT[:, :, cols],
                        ALU.mult, ALU.mult, [r_on, r_normw, r_srT], [r_catT])
                    tpm, r_tpm = PS()
                    mc, r_mc = (mfirst, r_mfirst) if it == 0 else (mcur, r_mcur)
                    for g in range(4):
                        MM(tpm[:, g * 128:(g + 1) * 128], ptok[par][:, g * 128:(g + 1) * 128], mc[:, g, :], True, False,
                           [r_ptok[par], r_mc], [r_tpm])
                        MM(tpm[:, g * 128:(g + 1) * 128], ptok[1 - par][:, g * 128:(g + 1) * 128], mprev[:, g, :], False, True,
                           [r_ptok[1 - par], r_mprev], [r_tpm])
                    ACT(mixT[:, :, :], tpm[:, 0:512].rearrange("p (a t) -> p a t", a=4), AF.Copy, [r_tpm], [r_mixT])
                    tpo, r_tpo = PS()
                    for g in range(4):
                        MM(tpo[:, g * 128:(g + 1) * 128], poolw_bf[:, g, :], mixT[:, g, :], True, True, [r_poolw, r_mixT], [r_tpo])
                    for g in range(4):
                        TS("dve", catT[:, g, :], tpo[:, g * 128:(g + 1) * 128], pscale_sb[:, g:g + 1], None, ALU.mult, None,
                           [r_tpo, r_pscale], [r_catT])
                    state_update(par, lambda half: ebT[:, half * 128 + 127:half * 128 + 128], r_ebT)
                    ACT(Sbf[:, :, :], S32[:, :, :], AF.Copy, [r_S32], [r_Sbf])
                    def part2(it=it, par=par, hcur=hcur, r_hcur=r_hcur):
                        tm0, r_tm0 = PS()
                        tm1, r_tm1 = PS()
                        for kc in range(8):
                            MM(tm0[:, 0:512], catT[:, kc, :], w_out_bf[:, kc, 0:512], kc == 0, kc == 7, [r_catT, r_wout], [r_tm0])
                            MM(tm1[:, 0:512], catT[:, kc, :], w_out_bf[:, kc, 512:1024], kc == 0, kc == 7, [r_catT, r_wout], [r_tm1])
                        STT(z1[:, 0:512], xt[par][:, 0:512], ALPHA, tm0[:, 0:512], ALU.mult, ALU.add, [r_xt[par], r_tm0], [r_z1])
                        STT(z1[:, 512:1024], xt[par][:, 512:1024], ALPHA, tm1[:, 0:512], ALU.mult, ALU.add, [r_xt[par], r_tm1], [r_z1])
                        layer_norm(z1, r_z1, g1, r_g1, b1, r_b1, hcur, r_hcur, lnt, r_lnt, st6, r_st6, mv, r_mv, sm, r_sm)
                    def route_a(it=it, cols=cols, hcur=hcur, r_hcur=r_hcur):
                        ACT(hb[:, :], hcur[:, :], AF.Copy, [r_hcur], [r_hb])
                        DMA("sp", hbf[it * 128:(it + 1) * 128, :], hb[:, :], [r_hb], [Res(f"hbf{it}", semgrp=g_hbf)])
                        tt0, r_tt0 = PS()
                        tt1, r_tt1 = PS()
                        for c in range(8):
                            tt, r_tt = (tt0, r_tt0) if c < 4 else (tt1, r_tt1)
                            TR(tt[:, (c % 4) * 128:(c % 4 + 1) * 128], hcur[:, c * 128:(c + 1) * 128], ident32[:, :],
                               [r_hcur, r_ident32], [r_tt])
                        CP("dve", hT32[:, 0:4, :], tt0[:, 0:512].rearrange("p (a t) -> p a t", a=4), [r_tt0], [r_hT32])
                        CP("dve", hT32[:, 4:8, :], tt1[:, 0:512].rearrange("p (a t) -> p a t", a=4), [r_tt1], [r_hT32])
                        ACT(hTb[:, 0:4, cols], tt0[:, 0:512].rearrange("p (a t) -> p a t", a=4), AF.Copy, [r_tt0], [r_hTb])
                        ACT(hTb[:, 4:8, cols], tt1[:, 0:512].rearrange("p (a t) -> p a t", a=4), AF.Copy, [r_tt1], [r_hTb])
                        trt, r_trt = PS()
                        for kc in range(8):
                            MM(trt[:, 0:256], hT32[:, kc, :], rw_sb[:, kc, :], kc == 0, kc == 7, [r_hT32, r_rw], [r_trt])
                        scores, r_scores = scores2[it % 2], r_scores2[it % 2]
                        ACT(scores[:, :], trt[:, 0:256], AF.Sigmoid, [r_trt], [r_scores])
                    def route_b(it=it):
                        scores, r_scores = scores2[it % 2], r_scores2[it % 2]
                        TT("dve", biased[:, :], scores[:, :], rbias_sb[:, :], ALU.add, [r_scores, r_rbias], [r_biased])
                        b3 = biased[:, :].rearrange("p (g i) -> p g i", g=8)
                        S.op("dve", lambda e, b3=b3: e.tensor_reduce(out=g8[:, 0:8], in_=b3, axis=AX.X, op=ALU.max), [r_biased], [r_g8])
                        TT("dve", tmpA[:, :].rearrange("p (g i) -> p g i", g=8), b3, g8[:, 0:8].unsqueeze(2).to_broadcast([128, 8, 32]),
                           ALU.is_equal, [r_biased, r_g8], [r_tmpA])
                        STT(tmpB[:, :], tmpA[:, :], -BIG, biased[:, :], ALU.mult, ALU.add, [r_tmpA, r_biased], [r_tmpB])
                        tb3 = tmpB[:, :].rearrange("p (g i) -> p g i", g=8)
                        S.op("dve", lambda e, tb3=tb3: e.tensor_reduce(out=g8[:, 8:16], in_=tb3, axis=AX.X, op=ALU.max), [r_tmpB], [r_g8])
                        TT("dve", g8[:, 8:16], g8[:, 8:16], g8[:, 0:8], ALU.add, [r_g8], [r_g8])
                        S.op("dve", lambda e: e.max(out=g8[:, 16:24], in_=g8[:, 8:16]), [r_g8], [r_g8])
                        TS("dve", g8[:, 24:32], g8[:, 8:16], g8[:, 19:20], None, ALU.is_ge, None, [r_g8], [r_g8])
                        TS("dve", g8[:, 24:32], g8[:, 24:32], -1.0, BIG, ALU.add, ALU.mult, [r_g8], [r_g8])
                        TT("dve", mbv[:, :].rearrange("p (g i) -> p g i", g=8), b3, g8[:, 24:32].unsqueeze(2).to_broadcast([128, 8, 32]),
                           ALU.add, [r_biased, r_g8], [r_mbv])
                        S.op("dve", lambda e: e.max(out=v8[:, :], in_=mbv[:, :]), [r_mbv], [r_v8])
                        TS("dve", sel[:, :], mbv[:, :], v8[:, 7:8], None, ALU.is_ge, None, [r_mbv, r_v8], [r_sel])
                        TT("dve", gs[:, :], sel[:, :], scores[:, :], ALU.mult, [r_sel, r_scores], [r_gs])
                        S.op("dve", lambda e: e.max(out=v8s[:, :], in_=gs[:, :]), [r_gs], [r_v8s])
                        S.op("dve", lambda e: e.tensor_reduce(out=rs[:, 0:1], in_=v8s[:, :], axis=AX.X, op=ALU.add), [r_v8s], [r_rs])
                        S.op("dve", lambda e: e.reciprocal(out=rs[:, 1:2], in_=rs[:, 0:1]), [r_rs], [r_rs])
                        TS("dve", w8[:, it * 8:(it + 1) * 8], v8s[:, :], rs[:, 1:2], 2.5, ALU.mult, ALU.mult, [r_v8s, r_rs], [r_w8])
                        tps, r_tps = PS()
                        MM(tps[:, 0:256], ones32[:, :], selcum[:, :], True, False, [r_ones, r_selcum], [r_tps])
                        MM(tps[:, 0:256], ustrict[:, :], sel[:, :], False, True, [r_ustrict, r_sel], [r_tps])
                        STT(slotfull[:, :], tps[:, 0:256], float(NE), iota_e[:, :], ALU.mult, ALU.add, [r_tps, r_iota], [r_slotfull])
                        TT("dve", selcum[:, :], selcum[:, :], sel[:, :], ALU.add, [r_selcum, r_sel], [r_selcum])
                        for k in range(8):
                            col = it * 8 + k
                            STT(junk[:, :], gs[:, :], v8s[:, k:k + 1], slotfull[:, :], ALU.is_equal, ALU.mult,
                                [r_gs, r_v8s, r_slotfull], [r_junk, r_slot8f], accum_out=slot8f[:, col:col + 1])
                        CP("dve", slot8i[:, it * 8:(it + 1) * 8], slot8f[:, it * 8:(it + 1) * 8], [r_slot8f], [r_slot8i])
                        for k in range(8):
                            col = it * 8 + k
                            S.op("pool", lambda e, col=col, it=it: e.indirect_dma_start(
                                out=slot_tok, out_offset=bass.IndirectOffsetOnAxis(ap=slot8i[:, col:col + 1], axis=0),
                                in_=tokid[:, it, :], in_offset=None, bounds_check=BC(e), oob_is_err=False),
                                [r_slot8i, r_tokid, r_slotinit, r_slotinit_b], [Res(f"slot{col}", semgrp=g_slot)], dma=True)

                    if stage != "h" and prev[0] is not None:
                        prev[0]()
                        if prev[2] is not None:
                            prev[2]()
                    part2()
                    if stage == "h":
                        DMA("sp", out[it * 128:(it + 1) * 128, :], hcur[:, :], [r_hcur], [Res(f"out{it}", semgrp=g_out)])
                        continue
                    if prev[1] is not None:
                        prev[1]()
                    prev[0], prev[1], prev[2] = route_a, route_b, None
                def shared_blk(blk=blk):
                    for j in range(2):
                        tg, r_tg = PS()
                        tu, r_tu = PS()
                        for kc in range(8):
                            MM(tg[:, 0:512], wsgu[:, kc, j * 128:(j + 1) * 128], hTb[:, kc, :], kc == 0, kc == 7, r_wsgu + [r_hTb], [r_tg])
                        for kc in range(8):
                            MM(tu[:, 0:512], wsgu[:, kc, 256 + j * 128:256 + (j + 1) * 128], hTb[:, kc, :], kc == 0, kc == 7,
                               r_wsgu + [r_hTb], [r_tu])
                        ACT(sg[:, :], tg[:, 0:512], AF.Silu, [r_tg], [r_sg])
                        TT("dve", actsh[:, j, :], sg[:, :], tu[:, 0:512], ALU.mult, [r_sg, r_tu], [r_actsh])
                    for t4 in range(4):
                        it = blk * 4 + t4
                        cols = slice(t4 * 128, (t4 + 1) * 128)
                        hcur, r_hcur = hring[it % 4], r_hring[it % 4]
                        ty0, r_ty0 = PS()
                        ty1, r_ty1 = PS()
                        for kc in range(2):
                            MM(ty0[:, 0:512], actsh[:, kc, cols], wsd_bf[:, kc, 0:512], kc == 0, kc == 1, [r_actsh, r_wsd], [r_ty0])
                            MM(ty1[:, 0:512], actsh[:, kc, cols], wsd_bf[:, kc, 512:1024], kc == 0, kc == 1, [r_actsh, r_wsd], [r_ty1])
                        zp = it % 2
                        STT(za[zp][:, 0:512], hcur[:, 0:512], ALPHA, ty0[:, 0:512], ALU.mult, ALU.add, [r_hcur, r_ty0], [r_za[zp]])
                        STT(za[zp][:, 512:1024], hcur[:, 512:1024], ALPHA, ty1[:, 0:512], ALU.mult, ALU.add, [r_hcur, r_ty1], [r_za[zp]])
                        DMA("sp", zacc[it * 128:(it + 1) * 128, :], za[zp][:, :], [r_za[zp]], [Res(f"zacc{it}", semgrp=g_zacc)])

                if stage != "h":
                    prev[2] = shared_blk
            if stage != "h":
                prev[0]()
                prev[2]()
                prev[1]()
            S.barrier()
            S.emit()

        if stage == "h":
            return nc

        with ExitStack() as st2:
            sb = mk(st2)
            st_v = slot_tok.rearrange("(s e) o -> s (e o)", e=NE)
            tbl, r_tbl = sb("tbl", [128, 2 * NE], I32), Res("tbl")
            tblB, r_tblB = sb("tblB", [128, 2 * NE], I32), Res("tblB")
            DMA("sp", tblB[:], c_tblinit, [], [r_tblB])
            DMA("sp", tblB[0:CAP - 128, :], st_v[128:CAP, :], [], [r_tblB])
            g2, r_g2 = sb("g2", [128, 1024]), Res("g2")
            DMA("sp", g2[:], ln2g, [], [r_g2])
            b2, r_b2 = sb("b2", [128, 1024]), Res("b2")
            DMA("sp", b2[:], ln2b, [], [r_b2])
            DMA("sp", tbl[:], st_v[0:128, :], [], [r_tbl])
            if stage == "route":
                f1 = DMA("sp", dbg_tbl, tbl[:], [r_tbl, r_tblB], [Res("dbg_tbl")])
                f2 = DMA("sp", dbg_slot, slot8f[:], [r_slot8f], [Res("dbg_slot")])
                f3 = DMA("sp", dbg_w8, w8[:], [r_w8], [Res("dbg_w8")])
                zt_, r_zt = sb("zt_", [128, 1024]), Res("zt_")
                fl = [f1, f2, f3]
                for it in range(NT):
                    DMA("sp", zt_[:, :], zacc[it * 128:(it + 1) * 128, :], [], [r_zt])
                    fl.append(DMA("sp", out[it * 128:(it + 1) * 128, :], zt_[:, :], [r_zt], [Res(f"out{it}", semgrp=g_out)]))
                S.wait_final("sp", fl)
                S.emit()
                return nc
            NB = 4
            wgu = [sb(f"wgu{i}", [128, 8, 512], BF16) for i in range(NB)]
            r_wg = [Res(f"wg{i}") for i in range(NB)]
            r_wu = [Res(f"wu{i}") for i in range(NB)]
            wd = [sb(f"wd{i}", [128, 2, 1024], BF16) for i in range(NB)]
            r_wd = [Res(f"wd{i}") for i in range(NB)]
            xg = [sb(f"xg{i}", [128, 1024], BF16) for i in range(2)]
            r_xg = [Res(f"xg{i}") for i in range(2)]
            for i in range(2):
                S.op("dve", lambda e, i=i: e.memset(xg[i][:], 0.0), [], [r_xg[i]])
            xgT = [sb(f"xgT{i}", [128, 8, 128], BF16) for i in range(2)]
            r_xgT = [Res(f"xgT{i}") for i in range(2)]
            sge = [sb(f"sge{i}", [128, 256]) for i in range(2)]
            r_sge = [Res(f"sge{i}") for i in range(2)]
            ae = [sb(f"ae{i}", [128, 256], BF16) for i in range(2)]
            r_ae = [Res(f"ae{i}") for i in range(2)]
            aT = [sb(f"aT{i}", [128, 2, 128], BF16) for i in range(2)]
            r_aT = [Res(f"aT{i}") for i in range(2)]
            NY = 4
            ysb = [sb(f"ysb{i}", [128, 1024]) for i in range(NY)]
            r_ysb = [Res(f"ysb{i}") for i in range(NY)]
            g_ys = [Res(f"yslots{i}") for i in range(NY)]
            r_ys_all = []
            ys_v = yslots.rearrange("(s e) d -> s e d", e=NE)

            def load_w(e_):
                wb = e_ % NB
                DMA("pool", wgu[wb][:, :, 0:256], weg[e_].rearrange("(c p) n -> p c n", p=128), [], [r_wg[wb]])
                DMA("pool", wgu[wb][:, :, 256:512], weu[e_].rearrange("(c p) n -> p c n", p=128), [], [r_wu[wb]])
                DMA("pool", wd[wb][:], wed[e_].rearrange("(c p) n -> p c n", p=128), [], [r_wd[wb]])

            def stage_a(e_, blk, j):
                wb = e_ % NB
                p2 = j % 2
                tb_src, r_tb_src = (tbl, r_tbl) if blk == 0 else (tblB, r_tblB)
                S.op("pool", lambda e: e.indirect_dma_start(
                    out=xg[p2][:, :], out_offset=None, in_=hbf,
                    in_offset=bass.IndirectOffsetOnAxis(ap=tb_src[:, 2 * e_:2 * e_ + 1], axis=0),
                    bounds_check=BCG(e), oob_is_err=False),
                    [r_tb_src], [r_xg[p2]], dma=True)
                t, r = PS()
                tb_ = t[:, :].bitcast(BF16)
                for c in range(8):
                    TR(tb_[:, c * 128:(c + 1) * 128], xg[p2][:, c * 128:(c + 1) * 128], identb[:, :], [r_xg[p2], r_identb], [r])
                CP("dve", xgT[p2][:, :, :], tb_[:, 0:1024].rearrange("p (a t) -> p a t", a=8), [r], [r_xgT[p2]])
                t2, r2 = PS()
                for kc in range(8):
                    MM(t2[:, 0:512], xgT[p2][:, kc, :], wgu[wb][:, kc, :], kc == 0, kc == 7, [r_xgT[p2], r_wg[wb], r_wu[wb]], [r2])
                ACT(sge[p2][:, :], t2[:, 0:256], AF.Silu, [r2], [r_sge[p2]])
                TT("dve", ae[p2][:, :], sge[p2][:, :], t2[:, 256:512], ALU.mult, [r_sge[p2], r2], [r_ae[p2]])

            def stage_b(e_, blk, j):
                wb = e_ % NB
                p2 = j % 2
                t, r = PS()
                tb_ = t[:, :].bitcast(BF16)
                for c in range(2):
                    TR(tb_[:, c * 128:(c + 1) * 128], ae[p2][:, c * 128:(c + 1) * 128], identb[:, :], [r_ae[p2], r_identb], [r])
                ACT(aT[p2][:, :, :], tb_[:, 0:256].rearrange("p (a t) -> p a t", a=2), AF.Copy, [r], [r_aT[p2]])
                ty0, r_ty0 = PS()
                ty1, r_ty1 = PS()
                for kc in range(2):
                    MM(ty0[:, 0:512], aT[p2][:, kc, :], wd[wb][:, kc, 0:512], kc == 0, kc == 1, [r_aT[p2], r_wd[wb]], [r_ty0])
                    MM(ty1[:, 0:512], aT[p2][:, kc, :], wd[wb][:, kc, 512:1024], kc == 0, kc == 1, [r_aT[p2], r_wd[wb]], [r_ty1])
                py = j % NY
                nr = 128 if blk == 0 else CAP - 128
                ACT(ysb[py][0:nr, 0:512], ty0[0:nr, 0:512], AF.Copy, [r_ty0], [r_ysb[py]])
                CP("dve", ysb[py][0:nr, 512:1024], ty1[0:nr, 0:512], [r_ty1], [r_ysb[py]])
                ry = Res(f"ys{e_}_{blk}", semgrp=g_ys[py])
                r_ys_all.append(ry)
                s0 = 0 if blk == 0 else 128
                DMA("sp", ys_v[s0:s0 + nr, e_, :], ysb[py][0:nr, :], [r_ysb[py]], [ry])

            work = [(e_, blk) for e_ in range(n_exp) for blk in range(2)]
            for j in range(len(work) + 1):
                if j < len(work):
                    if work[j][1] == 0:
                        load_w(work[j][0])
                    stage_a(work[j][0], work[j][1], j)
                if j >= 1:
                    stage_b(work[j - 1][0], work[j - 1][1], j - 1)

            acc = [sb(f"acc{i}", [128, 1024]) for i in range(2)]
            r_acc = [Res(f"acc{i}") for i in range(2)]
            NYK = 8
            yk = [sb(f"yk{i}", [128, 1024]) for i in range(NYK)]
            r_yk = [Res(f"yk{i}") for i in range(NYK)]
            lnt2, r_lnt2 = None, None
            ot = [sb(f"ot{i}", [128, 1024]) for i in range(2)]
            r_ot = [Res(f"ot{i}") for i in range(2)]
            st6b, r_st6b = sb("st6b", [128, 2, 6]), Res("st6b")
            mvb, r_mvb = sb("mvb", [128, 2]), Res("mvb")
            smb, r_smb = sb("smb", [128, 4]), Res("smb")
            finals = []
            gk = 0
            for it in range(NT):
                p2 = it % 2
                DMA("sp", acc[p2][:, :], zacc[it * 128:(it + 1) * 128, :], [], [r_acc[p2]])
                for k in range(8):
                    col = it * 8 + k
                    yb = gk % NYK
                    gk += 1
                    S.op("pool", lambda e, yb=yb, col=col: e.indirect_dma_start(
                        out=yk[yb][:, :], out_offset=None, in_=yslots,
                        in_offset=bass.IndirectOffsetOnAxis(ap=slot8i[:, col:col + 1], axis=0),
                        bounds_check=BC(e), oob_is_err=False),
                        [r_slot8i] + r_ys_all, [r_yk[yb]], dma=True)
                    STT(acc[p2][:, :], yk[yb][:, :], w8[:, col:col + 1], acc[p2][:, :], ALU.mult, ALU.add,
                        [r_yk[yb], r_w8, r_acc[p2]], [r_acc[p2]])
                layer_norm(acc[p2], r_acc[p2], g2, r_g2, b2, r_b2, ot[p2], r_ot[p2], lnt2, r_lnt2, st6b, r_st6b, mvb, r_mvb, smb, r_smb)
                finals.append(DMA("sp", out[it * 128:(it + 1) * 128, :], ot[p2][:, :], [r_ot[p2]], [Res(f"out{it}", semgrp=g_out2[p2])]))
            S.wait_final("sp", finals)
            S.emit()
    return nc


def _consts(half):
    c = {}
    tp = np.arange(128)[:, None]
    t = np.arange(128)[None, :]
    mcur = np.zeros((128, 4, 128), np.float32)
    mprev = np.zeros((128, 4, 128), np.float32)
    mfirst = np.zeros((128, 4, 128), np.float32)
    for g, w in enumerate((2, 4, 8, 16)):
        band = ((tp <= t) & (tp > t - w)).astype(np.float32)
        mcur[:, g, :] = band / w - (tp == t)
        mprev[:, g, :] = (((tp - 128) <= t) & ((tp - 128) > t - w)).astype(np.float32) / w
        cnt = np.minimum(t + 1, w).astype(np.float32)
        mfirst[:, g, :] = band / cnt - (tp == t)
    c["c_mcur"] = mcur
    c["c_mprev"] = mprev
    c["c_mfirst"] = mfirst if half == 0 else mcur
    c["c_uinc"] = (tp <= t).astype(np.float32) * (-1.0 / 16.0)
    c["c_rgt"] = (tp > t).astype(np.float32) * (-1.0 / 16.0)
    c["c_mask4"] = np.repeat((tp <= t).astype(np.float32)[:, None, :], 4, axis=1)
    c["c_ident"] = np.eye(128, dtype=np.float32)
    c["c_ustrict"] = (tp < t).astype(np.float32)
    c["c_ones"] = np.ones((128, 128), np.float32)
    c["c_iota"] = np.broadcast_to(np.arange(256, dtype=np.float32)[None, :], (128, 256)).copy()
    tk = (np.arange(NT, dtype=np.int32)[None, :] * 128 + np.arange(128, dtype=np.int32)[:, None]).astype(np.int32)
    c["c_tokid"] = np.repeat(tk[:, :, None], 2, axis=2)
    c["c_tblinit"] = np.full((128, 512), PAD_IDX, np.int32)
    return {k: np.ascontiguousarray(v) for k, v in c.items()}


def _relayout_gu(wg, wu):
    E = wg.shape[0]
    o = np.empty((E, 128, 8, 512), np.float32)
    o[:, :, :, 0:256] = wg.reshape(E, 8, 128, 256).transpose(0, 2, 1, 3)
    o[:, :, :, 256:512] = wu.reshape(E, 8, 128, 256).transpose(0, 2, 1, 3)
    return o.reshape(E, 128, 8 * 512)


def _relayout_d(wd):
    E = wd.shape[0]
    return np.ascontiguousarray(wd.reshape(E, 2, 128, 1024).transpose(0, 2, 1, 3)).reshape(E, 128, 2 * 1024)


def make_in_maps(x, w_in, gla_gate_w2, gla_gate_b, gla_norm_w, pool_w_group, pool_scale, w_out, ln1_g, ln1_b,
                 router_w, router_bias, w_exp_gate, w_exp_up, w_exp_down, w_sh_gate, w_sh_up, w_sh_down, ln2_g, ln2_b):
    f = lambda a: np.ascontiguousarray(np.asarray(a, dtype=np.float32))
    x = f(x)
    w_in0 = f(w_in)[0]
    wgl = np.zeros((1024, 128), np.float32)
    wgl[:, 0:16] = w_in0[:, 2048:2064]
    w2p = np.zeros((128, 256), np.float32)
    w2p[0:16] = f(gla_gate_w2)[0]
    w2p[32] = f(gla_gate_b)[0]
    bc = lambda v, n: np.ascontiguousarray(np.broadcast_to(f(v)[0][None, :], (128, n)))
    shared = {
        "w_in": w_in0, "wgl": wgl, "w2p": w2p,
        "normw": f(gla_norm_w)[0].reshape(128, 1).copy(),
        "poolw": f(pool_w_group)[0],
        "pscale": np.ascontiguousarray(f(pool_scale)[0].reshape(4, 128).T),
        "w_out": f(w_out)[0],
        "ln1g": bc(ln1_g, 1024), "ln1b": bc(ln1_b, 1024), "ln2g": bc(ln2_g, 1024), "ln2b": bc(ln2_b, 1024),
        "router_w": f(router_w)[0], "rbias": bc(router_bias, 256),
        "weg": f(w_exp_gate)[0], "weu": f(w_exp_up)[0], "wed": f(w_exp_down)[0],
        "wsg": f(w_sh_gate)[0], "wsu": f(w_sh_up)[0], "wsd": f(w_sh_down)[0],
    }
    cst = [_consts(0), _consts(1)]
    in_maps = []
    for c in range(NCORES):
        b, half = c // 2, c % 2
        xT = np.zeros((1024, 2 * TL), np.float32)
        if half == 1:
            xT[:, 0:TL] = x[b, 0:TL].T
        xT[:, TL:] = x[b, half * TL:(half + 1) * TL].T
        m = dict(shared)
        m.update(cst[half])
        m["xT"] = xT
        m["xtok"] = np.ascontiguousarray(x[b, half * TL:(half + 1) * TL])
        in_maps.append(m)
    return in_maps


_NC_CACHE = {}


def kernel(**inputs):
    in_maps = make_in_maps(**inputs)
    if "full" not in _NC_CACHE:
        _NC_CACHE["full"] = build("full")
    nc = _NC_CACHE["full"]
    res = run_bass_kernel_spmd(nc, in_maps, core_ids=list(range(NCORES)))
    outs = [np.asarray(r["out"], dtype=np.float32) for r in res.results]
    full = np.zeros((4, 4096, 1024), np.float32)
    for c in range(NCORES):
        b, half = c // 2, c % 2
        full[b, half * TL:(half + 1) * TL] = outs[c]
    return full
```
